# Optimizing a Trainium2 kernel written in Bass

```python
import math
import jax, jax.numpy as jnp
from jax import lax
import numpy as np

D_MODEL = 4096
BATCH = 4
SEQ = 2048
DEPTH = 1

N_MEM = 256
DIFF_WIDTH = D_MODEL // 2
DIFF_HEAD_DIM = 128
DIFF_V_DIM = 2 * DIFF_HEAD_DIM
DIFF_HEADS = DIFF_WIDTH // DIFF_V_DIM
DIFF_QK_WIDTH = DIFF_HEADS * 2 * DIFF_HEAD_DIM
CONV_WIDTH = D_MODEL // 4
CONV_TAPS = 31
MEM_WIDTH = D_MODEL // 4
MEM_HEADS = 4
MEM_HEAD_DIM = MEM_WIDTH // MEM_HEADS
MIX_WIDTH = DIFF_WIDTH + CONV_WIDTH + MEM_WIDTH
IN_SPLITS = [DIFF_QK_WIDTH, 2 * DIFF_QK_WIDTH, 2 * DIFF_QK_WIDTH + DIFF_WIDTH,
             2 * DIFF_QK_WIDTH + DIFF_WIDTH + 2 * CONV_WIDTH]
IN_COLS = IN_SPLITS[-1] + MEM_WIDTH
Q_BLOCK = 128
NUM_BUCKETS = 32
MAX_EXACT = NUM_BUCKETS // 2
MAX_DISTANCE = 128
N_EXPERTS = 32
TOP_K = 4
D_FF = 2048
SWIGLU_LIMIT = 7.0
SWIGLU_ALPHA = 1.702
EXPERT_BLOCK = 256
LN_EPS = 1e-5
DEEPNORM_ALPHA = (2 * DEPTH) ** 0.25
DEEPNORM_BETA = (8 * DEPTH) ** -0.25

kernel_name = "hymba_diffattn_conformer_memxattn_moe"


def layer_norm(x, g, b):
    xf = x.astype(jnp.float32)
    mu = jnp.mean(xf, -1, keepdims=True)
    var = jnp.mean(jnp.square(xf - mu), -1, keepdims=True)
    return ((xf - mu) * lax.rsqrt(var + LN_EPS) * g + b).astype(x.dtype)


def rms_norm(x, g):
    xf = x.astype(jnp.float32)
    return (xf * lax.rsqrt(jnp.mean(jnp.square(xf), -1, keepdims=True) + LN_EPS) * g).astype(x.dtype)


def t5_bucket(dist):
    n = jnp.maximum(dist, 0)
    nf = jnp.maximum(n, 1).astype(jnp.float32)
    large = MAX_EXACT + (jnp.log(nf / MAX_EXACT) / math.log(MAX_DISTANCE / MAX_EXACT)
                         * (NUM_BUCKETS - MAX_EXACT)).astype(jnp.int32)
    large = jnp.minimum(large, NUM_BUCKETS - 1)
    return jnp.where(n < MAX_EXACT, n, large)


def diff_attention(q, k, v, rel_table, lam, lam_init, subln_g):
    B, S, H = q.shape[:3]
    nb = S // Q_BLOCK
    qb = q.reshape(B, nb, Q_BLOCK, H, 2, DIFF_HEAD_DIM).transpose(1, 0, 2, 3, 4, 5)
    k_pos = jnp.arange(S)
    scale = DIFF_HEAD_DIM ** -0.5

    def block(args):
        q_blk, start = args
        dist = (start + jnp.arange(Q_BLOCK))[:, None] - k_pos[None, :]
        bias = rel_table[t5_bucket(dist)].transpose(2, 0, 1).astype(jnp.float32)
        logits = jnp.einsum('bqhcd,bkhcd->bchqk', q_blk, k).astype(jnp.float32) * scale + bias
        logits = jnp.where(dist >= 0, logits, -jnp.inf)
        probs = jax.nn.softmax(logits, axis=-1)
        w = probs[:, 0] - lam * probs[:, 1]
        return jnp.einsum('bhqk,bkhd->bqhd', w.astype(v.dtype), v)

    o = lax.map(block, (qb, jnp.arange(nb) * Q_BLOCK))
    o = o.transpose(1, 0, 2, 3, 4).reshape(B, S, H, DIFF_V_DIM)
    o = rms_norm(o, subln_g) * (1.0 - lam_init)
    return o.reshape(B, S, H * DIFF_V_DIM)


def conformer_conv(u, conv_w, conv_b, ln_g, ln_b):
    a, g = jnp.split(u, 2, axis=-1)
    h = a * jax.nn.sigmoid(g)
    h = lax.conv_general_dilated(h, conv_w, window_strides=(1,), padding=[(CONV_TAPS - 1, 0)],
                                 dimension_numbers=('NWC', 'WIO', 'NWC'),
                                 feature_group_count=CONV_WIDTH) + conv_b
    return jax.nn.silu(layer_norm(h, ln_g, ln_b))


def memory_attention(q, mem, w_mem_kv):
    B, S = q.shape[:2]
    M = mem.shape[1]
    k, v = jnp.split(mem @ w_mem_kv, 2, axis=-1)
    k = k.reshape(B, M, MEM_HEADS, MEM_HEAD_DIM)
    v = v.reshape(B, M, MEM_HEADS, MEM_HEAD_DIM)
    q = q.reshape(B, S, MEM_HEADS, MEM_HEAD_DIM)
    logits = jnp.einsum('bshd,bmhd->bhsm', q, k).astype(jnp.float32) * (MEM_HEAD_DIM ** -0.5)
    p = jax.nn.softmax(logits, axis=-1)
    return jnp.einsum('bhsm,bmhd->bshd', p.astype(v.dtype), v).reshape(B, S, MEM_WIDTH)


def moe(x, w_router, b_router, w_gu, b_gu, w_down, b_down):
    B, S, D = x.shape
    T = B * S
    xt = x.reshape(T, D)
    logits = (xt @ w_router + b_router).astype(jnp.float32)
    top_v, top_i = lax.top_k(logits, TOP_K)
    gates = jax.nn.softmax(top_v, axis=-1)
    n_assign = T * TOP_K
    n_blocks = -(-(n_assign + N_EXPERTS * (EXPERT_BLOCK - 1)) // EXPERT_BLOCK)
    flat_e = top_i.reshape(-1).astype(jnp.int32)
    order = jnp.argsort(flat_e)
    sorted_e = flat_e[order]
    counts = jnp.zeros((N_EXPERTS,), jnp.int32).at[flat_e].add(1)
    starts = jnp.cumsum(counts) - counts
    padded = (counts + EXPERT_BLOCK - 1) // EXPERT_BLOCK * EXPERT_BLOCK
    pad_ends = jnp.cumsum(padded)
    pad_starts = pad_ends - padded
    dest_sorted = pad_starts[sorted_e] + jnp.arange(n_assign, dtype=jnp.int32) - starts[sorted_e]
    buf_tok = jnp.full((n_blocks * EXPERT_BLOCK,), T, jnp.int32).at[dest_sorted].set(
        (order // TOP_K).astype(jnp.int32))
    dest = jnp.zeros((n_assign,), jnp.int32).at[order].set(dest_sorted)
    block_e = jnp.minimum(jnp.searchsorted(pad_ends, jnp.arange(n_blocks, dtype=jnp.int32) * EXPERT_BLOCK,
                                           side='right'), N_EXPERTS - 1)
    x_pad = jnp.concatenate([xt, jnp.zeros((1, D), xt.dtype)], axis=0)

    def expert_block(args):
        tok, e = args
        h = x_pad[tok] @ w_gu[e] + b_gu[e]
        gate, up = jnp.split(h, 2, axis=-1)
        gate = jnp.minimum(gate, SWIGLU_LIMIT)
        up = jnp.clip(up, -SWIGLU_LIMIT, SWIGLU_LIMIT)
        act = (up + 1.0) * (gate * jax.nn.sigmoid(SWIGLU_ALPHA * gate))
        return act @ w_down[e] + b_down[e]

    out = lax.map(expert_block, (buf_tok.reshape(n_blocks, EXPERT_BLOCK), block_e)).reshape(-1, D)
    y = jnp.einsum('tk,tkd->td', gates.astype(x.dtype), out[dest].reshape(T, TOP_K, D))
    return y.reshape(B, S, D)


def setup_inputs(seed: int = 0) -> dict:
    key = jax.random.key(seed)
    ks = iter(jax.random.split(key, 40))
    f32 = jnp.float32
    nrm = lambda shape, std: jax.random.normal(next(ks), shape, f32) * std
    D, L = D_MODEL, DEPTH
    sd = D ** -0.5
    w_in = jnp.concatenate([
        nrm((L, D, DIFF_QK_WIDTH), sd),
        nrm((L, D, DIFF_QK_WIDTH), sd),
        nrm((L, D, DIFF_WIDTH), sd * DEEPNORM_BETA),
        nrm((L, D, 2 * CONV_WIDTH), sd),
        nrm((L, D, MEM_WIDTH), sd),
    ], axis=-1)
    w_mem_kv = jnp.concatenate([nrm((L, D, MEM_WIDTH), sd),
                                nrm((L, D, MEM_WIDTH), sd * DEEPNORM_BETA)], axis=-1)
    return {
        "x": nrm((BATCH, SEQ, D), 1.0),
        "mem": nrm((BATCH, N_MEM, D), 1.0),
        "rel_table": nrm((NUM_BUCKETS, DIFF_HEADS), 0.5),
        "w_in": w_in,
        "w_mem_kv": w_mem_kv,
        "w_o": nrm((L, MIX_WIDTH, D), MIX_WIDTH ** -0.5 * DEEPNORM_BETA),
        "lambda_q1": nrm((L, DIFF_HEAD_DIM), 0.1),
        "lambda_k1": nrm((L, DIFF_HEAD_DIM), 0.1),
        "lambda_q2": nrm((L, DIFF_HEAD_DIM), 0.1),
        "lambda_k2": nrm((L, DIFF_HEAD_DIM), 0.1),
        "subln_g": 1.0 + nrm((L, DIFF_V_DIM), 0.02),
        "conv_w": nrm((L, CONV_TAPS, 1, CONV_WIDTH), CONV_TAPS ** -0.5),
        "conv_b": nrm((L, CONV_WIDTH), 0.02),
        "conv_ln_g": 1.0 + nrm((L, CONV_WIDTH), 0.02),
        "conv_ln_b": nrm((L, CONV_WIDTH), 0.02),
        "ln1_g": 1.0 + nrm((L, D), 0.02),
        "ln1_b": nrm((L, D), 0.02),
        "w_router": nrm((L, D, N_EXPERTS), sd),
        "b_router": nrm((L, N_EXPERTS), 0.01),
        "w_gate_up": nrm((L, N_EXPERTS, D, 2 * D_FF), sd),
        "b_gate_up": nrm((L, N_EXPERTS, 2 * D_FF), 0.02),
        "w_down": nrm((L, N_EXPERTS, D_FF, D), D_FF ** -0.5 * DEEPNORM_BETA),
        "b_down": nrm((L, N_EXPERTS, D), 0.02),
        "ln2_g": 1.0 + nrm((L, D), 0.02),
        "ln2_b": nrm((L, D), 0.02),
    }


def reference(x, mem, rel_table, w_in, w_mem_kv, w_o, lambda_q1, lambda_k1, lambda_q2, lambda_k2,
              subln_g, conv_w, conv_b, conv_ln_g, conv_ln_b, ln1_g, ln1_b, w_router, b_router,
              w_gate_up, b_gate_up, w_down, b_down, ln2_g, ln2_b):
    B, S, D = x.shape
    f32 = jnp.float32
    for l in range(DEPTH):
        lam_init = 0.8 - 0.6 * math.exp(-0.3 * l)
        lam = (jnp.exp(jnp.sum(lambda_q1[l].astype(f32) * lambda_k1[l].astype(f32)))
               - jnp.exp(jnp.sum(lambda_q2[l].astype(f32) * lambda_k2[l].astype(f32))) + lam_init)
        proj = x @ w_in[l]
        q_d, k_d, v_d, conv_in, q_m = jnp.split(proj, IN_SPLITS, axis=-1)
        a_out = diff_attention(q_d.reshape(B, S, DIFF_HEADS, 2, DIFF_HEAD_DIM),
                               k_d.reshape(B, S, DIFF_HEADS, 2, DIFF_HEAD_DIM),
                               v_d.reshape(B, S, DIFF_HEADS, DIFF_V_DIM),
                               rel_table, lam, lam_init, subln_g[l])
        c_out = conformer_conv(conv_in, conv_w[l], conv_b[l], conv_ln_g[l], conv_ln_b[l])
        m_out = memory_attention(q_m, mem, w_mem_kv[l])
        mix = jnp.concatenate([a_out, c_out, m_out], axis=-1) @ w_o[l]
        x = layer_norm(DEEPNORM_ALPHA * x + mix, ln1_g[l], ln1_b[l])
        ffn = moe(x, w_router[l], b_router[l], w_gate_up[l], b_gate_up[l], w_down[l], b_down[l])
        x = layer_norm(DEEPNORM_ALPHA * x + ffn, ln2_g[l], ln2_b[l])
    return x
```

```python
import math
from contextlib import ExitStack
import numpy as np
import concourse.bass as bass
import concourse.mybir as mybir
from concourse.bass_utils import run_bass_kernel_spmd

F32 = mybir.dt.float32
F32R = mybir.dt.float32r
I32 = mybir.dt.int32
U32 = mybir.dt.uint32
ALU = mybir.AluOpType
AF = mybir.ActivationFunctionType
AX = mybir.AxisListType

D = 4096
NE = 32
SLOTS = 256
ALPHA = 2.0 ** 0.25
LAM_INIT = 0.2
EPS = 1e-5
NEG = -30000.0
NEXP_UPLOAD = NE


def R(ap):
    return ap.bitcast(F32R)


class Prog:
    ENGS = ("pe", "act", "dve", "pool", "sp")

    def __init__(self, nc, stack):
        self.nc = nc
        self.stack = stack
        self.q = {e: [] for e in self.ENGS}
        self.psem = {}
        for e in ("pe", "act", "dve", "pool"):
            self.psem[e] = stack.enter_context(nc.semaphore("prog_" + e))
        self.cnt = {e: 0 for e in self.ENGS}
        self.waited = {e: {} for e in self.ENGS}
        self.state = {}
        self.dma_sems = {}
        self.dma_cnt = {}

    def _st(self, key):
        if key not in self.state:
            self.state[key] = {"w": None, "r": []}
        return self.state[key]

    def _prune(self, eng, deps):
        need = {}
        for sem, val in deps:
            k = id(sem)
            if self.waited[eng].get(k, 0) >= val:
                continue
            if k not in need or need[k][1] < val:
                need[k] = (sem, val)
        for k, (sem, val) in need.items():
            self.waited[eng][k] = val
        return list(need.values())

    def op(self, eng, fn, reads=(), writes=(), dsem=None):
        writes = list(writes) + [r for r in reads if r.startswith("ps") and r[2:].isdigit() and r not in writes]
        deps = []
        own = self.psem.get(eng) if dsem is None else None
        for r in reads:
            st = self._st(r)
            if st["w"] is not None:
                deps.append(st["w"])
        for w in writes:
            st = self._st(w)
            if st["w"] is not None and st["w"][0] is not own:
                deps.append(st["w"])
            for t in st["r"]:
                if t[0] is not own:
                    deps.append(t)
        deps = self._prune(eng, deps)
        if dsem is not None:
            if dsem not in self.dma_sems:
                self.dma_sems[dsem] = self.stack.enter_context(self.nc.semaphore("d_" + dsem))
                self.dma_cnt[dsem] = 0
            self.dma_cnt[dsem] += 16
            tok = (self.dma_sems[dsem], self.dma_cnt[dsem])
            inc = 16
        else:
            self.cnt[eng] += 1
            tok = (self.psem[eng], self.cnt[eng])
            inc = 1
        self.q[eng].append((deps, fn, tok[0], inc))
        for r in reads:
            self._st(r)["r"].append(tok)
        for w in writes:
            self.state[w] = {"w": tok, "r": []}
        return tok

    def barrier(self):
        toks = [(self.psem[e], self.cnt[e]) for e in ("pe", "act", "dve", "pool") if self.cnt[e] > 0]
        toks += [(self.dma_sems[n], self.dma_cnt[n]) for n in self.dma_sems]
        for e in self.ENGS:
            deps = self._prune(e, [t for t in toks if t[0] is not self.psem.get(e)])
            if deps:
                self.q[e].append((deps, None, None, 0))
        self.state = {}

    def emit(self):
        nc = self.nc
        q = self.q

        def run(e, items):
            for deps, fn, sem, inc in items:
                for s, v in deps:
                    e.wait_ge(s, v)
                if fn is not None:
                    fn(e).then_inc(sem, inc)

        with nc.Block() as block:
            @block.tensor
            def _(e):
                run(e, q["pe"])

            @block.scalar
            def _(e):
                run(e, q["act"])

            @block.vector
            def _(e):
                run(e, q["dve"])

            @block.gpsimd
            def _(e):
                run(e, q["pool"])

            @block.sync
            def _(e):
                run(e, q["sp"])
        self.q = {e: [] for e in self.ENGS}


def build(dbg=False, n_experts_run=NE, stop_after=None, n_heads=8, do_conv=True, do_mem=True, head_stage=3):
    nc = bass.Bass("TRN2", target_bir_lowering=False)
    nc.dge_precook = False

    def din(name, shape, dt=F32):
        return nc.dram_tensor(name, list(shape), dt, kind="ExternalInput")

    xkv_t = din("xkv", [16, 128, 32, 128]).ap()
    xq_t = din("xq", [8, 128, 32, 128]).ap()
    xh_t = din("xh", [128, 32, 32]).ap()
    xqT = din("xqT", [D, 1024]).ap()
    mem_t = din("memt", [2, 128, 32, 128]).ap()
    gvec_h = din("gvec", [8, 3072])
    w_in = din("w_in", [D, 9216]).ap().rearrange("(kc p) c -> p kc c", p=128)
    w_mkv = din("w_mkv", [D, 2048]).ap().rearrange("(kc p) c -> p kc c", p=128)
    w_o = din("w_o", [32, 128, 32 * 128]).ap()
    lamv_h = din("lamv", [1, 512])
    subln = din("subln", [128, 2]).ap()
    convw = din("convw", [128, 8, 31]).ap()
    convp = din("convp", [128, 3, 8]).ap()
    ln1p = din("ln1p", [128, 2, 32]).ap()
    w_rt = din("w_rt", [D, NE]).ap().rearrange("(kc p) c -> p kc c", p=128)
    b_rt = din("b_rt", [1, NE]).ap()
    w_gu = din("w_gu", [n_experts_run, D, D]).ap()
    b_gu = din("b_gu", [128, NE, 32]).ap()
    w_dn = din("w_dn", [n_experts_run, 2048, D]).ap()
    b_dn = din("b_dn", [NE, D]).ap()
    ln2p_h = din("ln2p", [2, D])
    out = nc.dram_tensor("out", [1024, D], F32, kind="ExternalOutput").ap()
    kd = "ExternalOutput" if dbg else "Internal"
    mixT = nc.dram_tensor("mixT", [D, 1024], F32, kind=kd).ap()
    x1d = nc.dram_tensor("x1d", [1024, D], F32, kind=kd).ap()
    Ed = nc.dram_tensor("Ed", [NE * SLOTS, D], F32, kind="Internal").ap()
    btok = nc.dram_tensor("btok", [NE * SLOTS, 1], I32, kind=kd).ap()

    with ExitStack() as top:
        P = Prog(nc, top)
        sbt = lambda st, name, shape, dt=F32: st.enter_context(nc.sbuf_tensor(name, list(shape), dt))
        pst = [top.enter_context(nc.psum_tensor("psb%d" % i, [128, 512], F32)) for i in range(8)]
        PK = ["ps%d" % i for i in range(8)]

        ident = sbt(top, "ident", [128, 128])
        ones = sbt(top, "ones", [128, 128])
        ustr = sbt(top, "ustr", [128, 128])
        neglam = sbt(top, "neglam", [128, 1])
        g8 = sbt(top, "g8", [128, 2])
        gates = sbt(top, "gates", [128, 8, 4])
        desti = sbt(top, "desti", [128, 8, 4], I32)

        cz = sbt(top, "cz", [128, 128])
        P.op("pool", lambda e: e.memset(cz[:], 0.0), writes=["cz"])
        P.op("pool", lambda e: e.affine_select(out=cz[:], in_=cz[:], pattern=[[-1, 128]],
                                               compare_op=ALU.not_equal, fill=1.0, base=0, channel_multiplier=1),
             reads=["cz"], writes=["cz"])
        P.op("dve", lambda e: e.tensor_copy(out=R(ident[:]), in_=cz[:]), reads=["cz"], writes=["ident"])
        P.op("pool", lambda e: e.memset(cz[:], 1.0), reads=[], writes=["cz"])
        P.op("dve", lambda e: e.tensor_copy(out=R(ones[:]), in_=cz[:]), reads=["cz"], writes=["ones"])
        P.op("pool", lambda e: e.affine_select(out=cz[:], in_=cz[:], pattern=[[1, 128]],
                                               compare_op=ALU.is_gt, fill=0.0, base=0, channel_multiplier=-1),
             reads=["cz"], writes=["cz"])
        P.op("dve", lambda e: e.tensor_copy(out=R(ustr[:]), in_=cz[:]), reads=["cz"], writes=["ustr"])


        def rsqrt_inplace(ap_fn, key, add_eps=False):
            if add_eps:
                P.op("dve", lambda e: e.tensor_scalar(out=ap_fn(), in0=ap_fn(), scalar1=EPS, scalar2=None, op0=ALU.add),
                     reads=[key], writes=[key])
            P.op("act", lambda e: e.activation(out=ap_fn(), in_=ap_fn(), func=AF.Sqrt), reads=[key], writes=[key])
            P.op("dve", lambda e: e.reciprocal(out=ap_fn(), in_=ap_fn()), reads=[key], writes=[key])

        def dma(eng, out_ap, in_ap, reads, writes, sem):
            return P.op(eng, lambda e, o=out_ap, i=in_ap: e.dma_start(out=o, in_=i), reads=reads, writes=writes, dsem=sem)


        pending = []

        def dma_kc(out3, in3, nk, step, writes, sem, eng="sp"):
            for k0 in range(0, nk, step):
                args = ("sp" if eng == "defer" else eng, R(out3[:, k0:k0 + step, :]), R(in3[:, k0:k0 + step, :]), [],
                        ["%s.%d" % (writes[0], k0)], sem)
                if eng == "defer":
                    pending.append(args)
                else:
                    dma(*args)

        def flush_pending(n=None):
            k = len(pending) if n is None else min(n, len(pending))
            for _ in range(k):
                dma(*pending.pop(0))

        def wk(base, nk=32, step=4):
            return ["%s.%d" % (base, k0) for k0 in range(0, nk, step)]

        with ExitStack() as ph:
            wkv = sbt(ph, "wkv", [128, 32, 512])
            wq = sbt(ph, "wq", [128, 32, 256])
            NXB = 2
            xb = [sbt(ph, "xb%d" % i, [128, 32, 128]) for i in range(NXB)]
            big = sbt(ph, "big", [128, 10240])
            KT = big[:, 0:4096].rearrange("p (m t) -> p m t", m=2)
            V = big[:, 4096:8192].rearrange("p (m t) -> p m t", m=16)
            QT = big[:, 8192:10240].rearrange("p (m t) -> p m t", m=2)
            hT = big[:, 0:8448].rearrange("p (m t) -> p m t", m=8)
            acc = wq[:].rearrange("p a b -> p (a b)").rearrange("p (m t) -> p m t", m=8)
            ksb = [sbt(ph, "ksb%d" % i, [128, 256]) for i in range(2)]
            btt = sbt(ph, "btt", [128, 2, 512])
            bt = [btt[:, i, :] for i in range(2)]
            xhb = btt[:].rearrange("p a b -> p (a b)").rearrange("p (k t) -> p k t", k=32)
            pt = [sbt(ph, "pt%d" % i, [128, 512]) for i in range(2)]
            rec = sbt(ph, "rec", [128, 512])
            o1 = sbt(ph, "o1", [128, 2, 512])
            of = sbt(ph, "of", [128, 2, 512])
            sq = sbt(ph, "sq", [128, 2, 512])
            rstd = sbt(ph, "rstd", [128, 512])
            ao = [sbt(ph, "ao%d" % i, [128, 2, 512]) for i in range(2)]
            lv = of[:, 0, :]
            lpr = of[:, 1, 0:256]
            ls = sbt(ph, "ls", [128, 4])
            sub_sb = sbt(ph, "sub_sb", [128, 2])
            cw = sbt(ph, "cw", [128, 8, 31])
            cp = sbt(ph, "cp", [128, 3, 8])
            sig = [pt[i][:, 0:256] for i in range(2)]
            mean = rec
            tmp = o1[:, 0, :]

            dma("sp", lv[:], bass.AP(lamv_h, 0, [[0, 128], [1, 512]]), [], ["lv"], "misc")
            dma("sp", sub_sb[:], subln, [], ["sub_sb"], "misc2")
            dma("sp", cw[:], convw, [], ["cw"], "misc3")
            dma("sp", cp[:], convp, [], ["cp"], "misc4")
            P.op("dve", lambda e: e.tensor_tensor(out=lpr[:, 0:128], in0=lv[:, 0:128], in1=lv[:, 128:256], op=ALU.mult),
                 reads=["lv"], writes=["lpr0"])
            P.op("dve", lambda e: e.tensor_tensor(out=lpr[:, 128:256], in0=lv[:, 256:384], in1=lv[:, 384:512], op=ALU.mult),
                 reads=["lv"], writes=["lpr1"])
            P.op("dve", lambda e: e.reduce_sum(out=ls[:, 0:1], in_=lpr[:, 0:128], axis=AX.X), reads=["lpr0"], writes=["ls0"])
            P.op("dve", lambda e: e.reduce_sum(out=ls[:, 1:2], in_=lpr[:, 128:256], axis=AX.X), reads=["lpr1"], writes=["ls1"])
            P.op("act", lambda e: e.activation(out=ls[:, 2:4], in_=ls[:, 0:2], func=AF.Exp), reads=["ls0", "ls1"], writes=["ls2"])
            P.op("dve", lambda e: e.tensor_tensor(out=neglam[:], in0=ls[:, 3:4], in1=ls[:, 2:3], op=ALU.subtract),
                 reads=["ls2"], writes=["neglam"])
            P.op("dve", lambda e: e.tensor_scalar(out=neglam[:], in0=neglam[:], scalar1=-LAM_INIT, scalar2=None, op0=ALU.add),
                 reads=["neglam"], writes=["neglam"])
            P.op("dve", lambda e: e.tensor_scalar(out=g8[:], in0=sub_sb[:], scalar1=1.0 - LAM_INIT, scalar2=None, op0=ALU.mult),
                 reads=["sub_sb"], writes=["g8"])

            xcnt = [0]

            def proj(x_tiles, n_tiles, w_tile, wkey, N, evac, halo=False):
                wkeys = (wk("wkvA") + wk("wkvB")) if wkey == "wkv" else wk(wkey)
                for t in range(n_tiles):
                    i = xcnt[0] % NXB
                    ip = xcnt[0] % 2
                    xcnt[0] += 1
                    xkey = "xb%d" % i
                    pkey = PK[ip]
                    ps = pst[ip]
                    dma("sp", R(xb[i][:].rearrange("p k t -> p (k t)")), R(x_tiles[t].rearrange("p k t -> p (k t)")), [], [xkey], xkey)

                    def mm(e, i=i, ps=ps):
                        for kc in range(32):
                            ins = e.matmul(ps[:, 0:N], lhsT=R(xb[i][:, kc, :]), rhs=R(w_tile[:, kc, 0:N]),
                                           start=(kc == 0), stop=(kc == 31))
                        return ins
                    P.op("pe", mm, reads=[xkey] + wkeys, writes=[pkey])
                    evac(t, ps, pkey, 128)
                if halo:
                    dma("sp", R(xhb[:].rearrange("p k t -> p (k t)")), R(xh_t.rearrange("p k t -> p (k t)")), [], ["xhb"], "xhb")
                    ps = pst[2]

                    def mmh(e, ps=ps):
                        for kc in range(32):
                            ins = e.matmul(ps[0:32, 0:N], lhsT=R(xhb[:, kc, :]), rhs=R(w_tile[:, kc, 0:N]),
                                           start=(kc == 0), stop=(kc == 31))
                        return ins
                    P.op("pe", mmh, reads=["xhb"] + wkeys, writes=[PK[2]])
                    evac(-1, ps, PK[2], 32)

            tcnt = [0, 0]

            def transpose_to(src_sb, src_key, dst_ap_fn, dst_key, np_=128, scale_eng="dve"):
                ps = pst[3]

                def tr(e):
                    for m in range(2):
                        ins = e.transpose(ps[:, m * 128:m * 128 + np_], src_sb[0:np_, m * 128:(m + 1) * 128],
                                          ident[0:np_, 0:np_])
                    return ins
                P.op("pe", tr, reads=[src_key, "ident"], writes=[PK[3]])
                src = ps[:, 0:256].rearrange("p (m t) -> p m t", m=2)[:, :, 0:np_]
                P.op(scale_eng, lambda e: e.tensor_copy(out=R(dst_ap_fn()), in_=src), reads=[PK[3]], writes=[dst_key])

            def attention(hd_rows, n_dc, nkt_fn, bias_hd, nmaps, post):
                accb = [(6, 7, 2), (0, 1, 3)]
                for c in range(2):
                    nk = nkt_fn(c)

                    def emit_lg(j, c=c):
                        slots = []
                        bj = None
                        if bias_hd is not None:
                            bj = tcnt[1] % 2
                            tcnt[1] += 1
                            off = 1920 + 512 * c - 128 * j
                            dma("sp", R(bt[bj][:]), R(bass.AP(gvec_h, bias_hd * 3072 + off, [[1, 128], [1, 512]])),
                                [], ["bt%d" % bj], "bt%d" % bj)
                            flush_pending(1)
                        for m in range(nmaps):
                            jj = tcnt[0] % 2
                            tcnt[0] += 1
                            psl = pst[4 + jj]
                            plk = PK[4 + jj]
                            if bias_hd is not None:
                                def lg(e, psl=psl, m=m, j=j, bj=bj):
                                    e.matmul(psl[:], lhsT=R(KT[:, m, j * 128:(j + 1) * 128]),
                                             rhs=R(QT[:, m, c * 512:(c + 1) * 512]), start=True, stop=False)
                                    return e.matmul(psl[:], lhsT=R(ident[:]), rhs=R(bt[bj][:]), start=False, stop=True)
                                P.op("pe", lg, reads=["KT", "QT", "bt%d" % bj, "ident"], writes=[plk])
                            else:
                                def lg(e, psl=psl, j=j):
                                    for dc in range(n_dc):
                                        ins = e.matmul(psl[:], lhsT=R(KT[:, dc, j * 128:(j + 1) * 128]),
                                                       rhs=R(QT[:, dc, c * 512:(c + 1) * 512]),
                                                       start=(dc == 0), stop=(dc == n_dc - 1))
                                    return ins
                                P.op("pe", lg, reads=["KT", "QT"], writes=[plk])
                            P.op("act", lambda e, jj=jj, psl=psl: e.activation(out=R(pt[jj][:]), in_=psl[:], func=AF.Exp),
                                 reads=[plk], writes=["pt%d" % jj])
                            slots.append(jj)
                        return slots

                    def emit_pv(j, slots, nk=nk):
                        for m in range(nmaps):
                            jj = slots[m]
                            b0, b1, b2 = accb[m]

                            def pv(e, j=j, jj=jj, b0=b0, b1=b1, b2=b2):
                                e.matmul(pst[b0][:], lhsT=R(V[:, j, 0:128]), rhs=R(pt[jj][:]), start=(j == 0), stop=(j == nk - 1))
                                e.matmul(pst[b1][:], lhsT=R(V[:, j, 128:256]), rhs=R(pt[jj][:]), start=(j == 0), stop=(j == nk - 1))
                                return e.matmul(pst[b2][:], lhsT=R(ones[:]), rhs=R(pt[jj][:]), start=(j == 0), stop=(j == nk - 1))
                            P.op("pe", pv, reads=["V", "pt%d" % jj, "ones"], writes=[PK[b0], PK[b1], PK[b2]])

                    if nmaps == 2:
                        prev = emit_lg(0)
                        for j in range(nk):
                            emit_pv(j, prev)
                            prev = emit_lg(j + 1) if j + 1 < nk else None
                    else:
                        prev = emit_lg(0)
                        for j in range(nk):
                            nxt = emit_lg(j + 1) if j + 1 < nk else None
                            emit_pv(j, prev)
                            prev = nxt
                    for m in range(nmaps):
                        b0, b1, b2 = accb[m]
                        P.op("dve", lambda e, b2=b2: e.reciprocal(out=rec[:], in_=pst[b2][:]), reads=[PK[b2]], writes=["rec"])
                        dst = o1 if (nmaps == 2 and m == 0) else of
                        dk = "o1" if (nmaps == 2 and m == 0) else "of"
                        for dvc, bb in enumerate((b0, b1)):
                            P.op("dve", lambda e, dvc=dvc, dst=dst, bb=bb: e.tensor_tensor(
                                out=dst[:, dvc, :], in0=pst[bb][:], in1=rec[:], op=ALU.mult),
                                reads=[PK[bb], "rec"], writes=[dk + str(dvc)])
                    post(c)
                flush_pending()

            ocnt = [0]

            def store_mix(row0, c, src_tile, src_keys):
                for dvc in range(2):
                    dma("act", mixT[row0 + dvc * 128: row0 + (dvc + 1) * 128, c * 512:(c + 1) * 512],
                        src_tile[:, dvc, :], [src_keys[dvc]], [], "st_" + src_keys[dvc])

            def load_head_w(hd, eng="sp"):
                dma_kc(wkv[:, :, 0:256], w_in[:, :, 2048 + hd * 256: 2048 + (hd + 1) * 256], 32, 4, ["wkvA"], "wkv", eng)
                dma_kc(wkv[:, :, 256:512], w_in[:, :, 4096 + hd * 256: 4096 + (hd + 1) * 256], 32, 4, ["wkvB"], "wkv", eng)
                dma_kc(wq, w_in[:, :, hd * 256:(hd + 1) * 256], 32, 4, ["wq"], "wq", eng)

            for hd in range(n_heads):
                if hd == 0:
                    load_head_w(0)

                def evac_kv(t, ps, pkey, np_):
                    i = t % 2
                    P.op("act", lambda e, i=i, ps=ps: e.copy(out=ksb[i][:], in_=ps[:, 0:256]), reads=[pkey], writes=["ksb%d" % i])
                    P.op("dve", lambda e, t=t, ps=ps: e.tensor_copy(out=R(V[:, t, :]), in_=ps[:, 256:512]), reads=[pkey], writes=["V"])
                    transpose_to(ksb[i], "ksb%d" % i, lambda t=t: KT[:, :, t * 128:(t + 1) * 128], "KT")
                proj(xkv_t, 16, wkv, "wkv", 512, evac_kv)

                def evac_q(t, ps, pkey, np_):
                    i = t % 2
                    P.op("act", lambda e, i=i, ps=ps: e.mul(out=ksb[i][:], in_=ps[:, 0:256], mul=128.0 ** -0.5),
                         reads=[pkey], writes=["ksb%d" % i])
                    transpose_to(ksb[i], "ksb%d" % i, lambda t=t: QT[:, :, t * 128:(t + 1) * 128], "QT")
                if head_stage >= 2:
                    proj(xq_t, 8, wq, "wq", 256, evac_q)

                def post_diff(c, hd=hd):
                    for dvc in range(2):
                        P.op("dve", lambda e, dvc=dvc: e.scalar_tensor_tensor(
                            out=of[:, dvc, :], in0=of[:, dvc, :], scalar=neglam[:, 0:1], in1=o1[:, dvc, :],
                            op0=ALU.mult, op1=ALU.add), reads=["of%d" % dvc, "o1%d" % dvc, "neglam"], writes=["of%d" % dvc])
                        P.op("act", lambda e, dvc=dvc: e.activation(out=R(sq[:, dvc, :]), in_=of[:, dvc, :], func=AF.Square),
                             reads=["of%d" % dvc], writes=["sq%d" % dvc])

                    def ssum(e):
                        e.matmul(pst[3][:], lhsT=R(ones[:]), rhs=R(sq[:, 0, :]), start=True, stop=False)
                        return e.matmul(pst[3][:], lhsT=R(ones[:]), rhs=R(sq[:, 1, :]), start=False, stop=True)
                    P.op("pe", ssum, reads=["sq0", "sq1", "ones"], writes=[PK[3]])
                    P.op("dve", lambda e: e.tensor_scalar(out=rstd[:], in0=pst[3][:], scalar1=1.0 / 256, scalar2=EPS,
                                                          op0=ALU.mult, op1=ALU.add), reads=[PK[3]], writes=["rstd"])
                    rsqrt_inplace(lambda: rstd[:], "rstd")
                    a = ao[ocnt[0] % 2]
                    ak = "ao%d_" % (ocnt[0] % 2)
                    ocnt[0] += 1
                    for dvc in range(2):
                        P.op("dve", lambda e, dvc=dvc, a=a: e.scalar_tensor_tensor(
                            out=a[:, dvc, :], in0=of[:, dvc, :], scalar=g8[:, dvc:dvc + 1], in1=rstd[:],
                            op0=ALU.mult, op1=ALU.mult), reads=["of%d" % dvc, "g8", "rstd"], writes=[ak + str(dvc)])
                    store_mix(hd * 256, c, a, [ak + "0", ak + "1"])

                if hd + 1 < n_heads:
                    load_head_w(hd + 1, "defer")
                if head_stage >= 3:
                    attention(hd * 256, 1, lambda c: 12 + 4 * c, hd, 2, post_diff)

            P.barrier()
            for ci in range(4 if do_conv else 0):
                dma_kc(wkv[:, :, 0:256], w_in[:, :, 6144 + ci * 256: 6144 + (ci + 1) * 256], 32, 4, ["wkvA"], "wkv")
                dma_kc(wkv[:, :, 256:512], w_in[:, :, 7168 + ci * 256: 7168 + (ci + 1) * 256], 32, 4, ["wkvB"], "wkv")

                def evac_glu(t, ps, pkey, np_, ci=ci):
                    i = (t + 2) % 2
                    P.op("act", lambda e, i=i, ps=ps: e.activation(out=R(sig[i][0:np_, :]), in_=ps[0:np_, 256:512], func=AF.Sigmoid),
                         reads=[pkey], writes=["pt%d" % i])
                    P.op("dve", lambda e, i=i, ps=ps: e.tensor_tensor(out=ksb[i][0:np_, :], in0=ps[0:np_, 0:256], in1=sig[i][0:np_, :],
                                                                     op=ALU.mult), reads=[pkey, "pt%d" % i], writes=["ksb%d" % i])
                    if t >= 0:
                        dst = lambda t=t: hT[:, 2 * ci:2 * ci + 2, 32 + t * 128: 32 + (t + 1) * 128]
                    else:
                        dst = lambda: hT[:, 2 * ci:2 * ci + 2, 0:32]
                    transpose_to(ksb[i], "ksb%d" % i, dst, "hT%d" % ci, np_=np_)
                proj(xq_t, 8, wkv, "wkv", 512, evac_glu, halo=True)

            for ch in range(8 if do_conv else 0):
                eng = "dve"
                hk = "hT%d" % (ch // 2)
                akey = "acc%d" % ch
                P.op(eng, lambda e, ch=ch: e.tensor_scalar(out=R(acc[:, ch, :]), in0=hT[:, ch, 2:1026], scalar1=cw[:, ch, 0:1],
                                                           scalar2=None, op0=ALU.mult), reads=[hk, "cw"], writes=[akey])
                for j in range(1, 31):
                    P.op(eng, lambda e, ch=ch, j=j: e.scalar_tensor_tensor(
                        out=R(acc[:, ch, :]), in0=hT[:, ch, 2 + j:1026 + j], scalar=cw[:, ch, j:j + 1], in1=acc[:, ch, :],
                        op0=ALU.mult, op1=ALU.add), reads=[hk, "cw", akey], writes=[akey])
                P.op(eng, lambda e, ch=ch: e.tensor_scalar(out=R(acc[:, ch, :]), in0=acc[:, ch, :], scalar1=cp[:, 0, ch:ch + 1],
                                                           scalar2=None, op0=ALU.add), reads=[akey, "cp"], writes=[akey])
            for c in range(2 if do_conv else 0):
                cs = slice(c * 512, (c + 1) * 512)
                for ch in range(8):
                    P.op("act", lambda e, ch=ch, cs=cs: e.activation(out=R(sq[:, ch % 2, :]), in_=acc[:, ch, cs], func=AF.Square),
                         reads=["acc%d" % ch], writes=["sq%d" % (ch % 2)])
                    P.op("pe", lambda e, ch=ch, cs=cs: e.matmul(pst[2][:], lhsT=R(ones[:]), rhs=R(acc[:, ch, cs]),
                                                                start=(ch == 0), stop=(ch == 7)),
                         reads=["acc%d" % ch, "ones"], writes=[PK[2]])
                    P.op("pe", lambda e, ch=ch: e.matmul(pst[3][:], lhsT=R(ones[:]), rhs=R(sq[:, ch % 2, :]),
                                                         start=(ch == 0), stop=(ch == 7)),
                         reads=["sq%d" % (ch % 2), "ones"], writes=[PK[3]])
                P.op("dve", lambda e: e.tensor_scalar(out=mean[:], in0=pst[2][:], scalar1=1.0 / 1024, scalar2=None, op0=ALU.mult),
                     reads=[PK[2]], writes=["mean"])
                P.op("dve", lambda e: e.tensor_tensor(out=tmp[:], in0=mean[:], in1=mean[:], op=ALU.mult), reads=["mean"], writes=["tmp"])
                P.op("dve", lambda e: e.scalar_tensor_tensor(out=rstd[:], in0=pst[3][:], scalar=1.0 / 1024, in1=tmp[:],
                                                             op0=ALU.mult, op1=ALU.subtract), reads=[PK[3], "tmp"], writes=["rstd"])
                rsqrt_inplace(lambda rstd=rstd: rstd[:], "rstd", add_eps=True)
                for ch in range(8):
                    a = ao[ocnt[0] % 2]
                    ak = "ao%d_" % (ocnt[0] % 2)
                    if ch % 2 == 1:
                        ocnt[0] += 1
                    d2 = ch % 2
                    P.op("dve", lambda e, ch=ch, cs=cs: e.tensor_tensor(out=tmp[:], in0=acc[:, ch, cs], in1=mean[:], op=ALU.subtract),
                         reads=["acc%d" % ch, "mean"], writes=["tmp"])
                    P.op("dve", lambda e: e.tensor_tensor(out=tmp[:], in0=tmp[:], in1=rstd[:], op=ALU.mult),
                         reads=["tmp", "rstd"], writes=["tmp"])
                    P.op("act", lambda e, ch=ch, a=a, d2=d2: e.activation(out=a[:, d2, :], in_=tmp[:], func=AF.Silu,
                                                                         bias=cp[:, 2, ch:ch + 1], scale=cp[:, 1, ch:ch + 1]),
                         reads=["tmp", "cp"], writes=[ak + str(d2)])
                    dma("act", mixT[2048 + ch * 128: 2048 + (ch + 1) * 128, cs], a[:, d2, :], [ak + str(d2)], [], "st_" + ak + str(d2))

            P.barrier()
            for hd in range(4 if do_mem else 0):
                dma_kc(wkv[:, :, 0:256], w_mkv[:, :, hd * 256:(hd + 1) * 256], 32, 4, ["wkvA"], "wkv")
                dma_kc(wkv[:, :, 256:512], w_mkv[:, :, 1024 + hd * 256: 1024 + (hd + 1) * 256], 32, 4, ["wkvB"], "wkv")
                dma_kc(wq, w_in[:, :, 8192 + hd * 256: 8192 + (hd + 1) * 256], 32, 4, ["wq"], "wq")

                def evac_mkv(t, ps, pkey, np_):
                    i = t % 2
                    P.op("act", lambda e, i=i, ps=ps: e.copy(out=ksb[i][:], in_=ps[:, 0:256]), reads=[pkey], writes=["ksb%d" % i])
                    P.op("dve", lambda e, t=t, ps=ps: e.tensor_copy(out=R(V[:, t, :]), in_=ps[:, 256:512]), reads=[pkey], writes=["V"])
                    transpose_to(ksb[i], "ksb%d" % i, lambda t=t: KT[:, :, t * 128:(t + 1) * 128], "KT")
                proj(mem_t, 2, wkv, "wkv", 512, evac_mkv)

                def evac_mq(t, ps, pkey, np_):
                    i = t % 2
                    P.op("act", lambda e, i=i, ps=ps: e.mul(out=ksb[i][:], in_=ps[:, 0:256], mul=256.0 ** -0.5),
                         reads=[pkey], writes=["ksb%d" % i])
                    transpose_to(ksb[i], "ksb%d" % i, lambda t=t: QT[:, :, t * 128:(t + 1) * 128], "QT")
                proj(xq_t, 8, wq, "wq", 256, evac_mq)

                def post_mem(c, hd=hd):
                    a = ao[ocnt[0] % 2]
                    ak = "ao%d_" % (ocnt[0] % 2)
                    ocnt[0] += 1
                    for dvc in range(2):
                        P.op("act", lambda e, dvc=dvc, a=a: e.copy(out=a[:, dvc, :], in_=of[:, dvc, :]),
                             reads=["of%d" % dvc], writes=[ak + str(dvc)])
                    store_mix(3072 + hd * 256, c, a, [ak + "0", ak + "1"])
                attention(0, 2, lambda c: 2, None, 1, post_mem)

            P.barrier()
            P.emit()

        if stop_after == 'A':
            return nc
        with ExitStack() as ph:
            mix = sbt(ph, "mix", [128, 32, 512])
            yT = sbt(ph, "yT", [128, 32, 512])
            wo = [sbt(ph, "wo%d" % i, [128, 32, 128]) for i in range(2)]
            xt = [sbt(ph, "xt%d" % i, [128, 512]) for i in range(2)]
            sqb = [sbt(ph, "sqb%d" % i, [128, 512]) for i in range(2)]
            mean = sbt(ph, "mean1", [128, 512])
            rstd = sbt(ph, "rstd1", [128, 512])
            tmp = [sbt(ph, "tmp1_%d" % i, [128, 512]) for i in range(2)]
            l1 = sbt(ph, "l1", [128, 2, 32])
            wr = sbt(ph, "wr", [128, 32, NE])
            brt = sbt(ph, "brt", [1, NE])
            x1s = [sbt(ph, "x1s0", [128, D])]
            lg = sbt(ph, "lg", [128, NE])
            mx8 = sbt(ph, "mx8", [128, 8, 8])
            mi8 = sbt(ph, "mi8", [128, 8, 8], U32)
            idxf = sbt(ph, "idxf", [128, 8, 4])
            negm = sbt(ph, "negm", [128, 8])
            esum = sbt(ph, "esum", [128, 8])
            maskall = sbt(ph, "maskall", [128, 8, NE])
            posall = sbt(ph, "posall", [128, 8, NE])
            iot = sbt(ph, "iot", [128, NE])
            oh = sbt(ph, "oh", [128, NE])
            possel = sbt(ph, "possel", [128, 8, 4])
            destf = sbt(ph, "destf", [128, 8, 4])
            tokid = sbt(ph, "tokid", [128, 8], I32)
            zt = sbt(ph, "zt", [128, 64], I32)

            dma("sp", l1[:], ln1p, [], ["l1"], "c_l1")
            dma_kc(wr, w_rt, 32, 4, ["wr"], "c_wr")
            dma("sp", R(brt[:]), R(b_rt), [], ["brt"], "c_brt")
            P.op("pool", lambda e: e.iota(iot[:], pattern=[[1, NE]], base=0, channel_multiplier=0,
                                           allow_small_or_imprecise_dtypes=True), writes=["iot"])
            P.op("pool", lambda e: e.iota(tokid[:], pattern=[[128, 8]], base=0, channel_multiplier=1), writes=["tokid"])
            P.op("pool", lambda e: e.memset(zt[:], 0), writes=["zt"])
            dma("sp", btok.rearrange("(p f) o -> p (f o)", p=128), zt[:], ["zt"], ["btok"], "c_bt")

            wcnt = 0
            x1cnt = 0
            for tg in range(2):
                ts_ = slice(tg * 512, (tg + 1) * 512)
                for q4 in range(8):
                    dma("sp", R(mix[:, q4 * 4:(q4 + 1) * 4, :]),
                        R(mixT[q4 * 512:(q4 + 1) * 512, ts_].rearrange("(kc p) t -> p kc t", p=128)),
                        [], ["mix.%d" % q4], "c_mix")
                for cb in range(32):
                    i = wcnt % 2
                    wcnt += 1
                    dma("sp", R(wo[i][:].rearrange("p k c -> p (k c)")), R(w_o[cb]), [], ["wo%d" % i], "c_wo%d" % i)
                    dma("sp", xt[i][:], xqT[cb * 128:(cb + 1) * 128, ts_], [], ["xt%d" % i], "c_xt%d" % i)
                    ps = pst[i]

                    def mmo(e, i=i, ps=ps):
                        for kc in range(32):
                            ins = e.matmul(ps[:], lhsT=R(wo[i][:, kc, :]), rhs=R(mix[:, kc, :]), start=(kc == 0), stop=(kc == 31))
                        return ins
                    P.op("pe", mmo, reads=["wo%d" % i] + ["mix.%d" % q for q in range(8)], writes=[PK[i]])
                    P.op("dve", lambda e, i=i, ps=ps, cb=cb: e.scalar_tensor_tensor(
                        out=R(yT[:, cb, :]), in0=xt[i][:], scalar=ALPHA, in1=ps[:], op0=ALU.mult, op1=ALU.add),
                        reads=["xt%d" % i, PK[i]], writes=["yT%d" % cb])
                    P.op("act", lambda e, i=i, cb=cb: e.activation(out=R(sqb[i][:]), in_=yT[:, cb, :], func=AF.Square),
                         reads=["yT%d" % cb], writes=["sqb%d" % i])
                    P.op("pe", lambda e, cb=cb: e.matmul(pst[2][:], lhsT=R(ones[:]), rhs=R(yT[:, cb, :]), start=(cb == 0), stop=(cb == 31)),
                         reads=["yT%d" % cb, "ones"], writes=[PK[2]])
                    P.op("pe", lambda e, cb=cb, i=i: e.matmul(pst[3][:], lhsT=R(ones[:]), rhs=R(sqb[i][:]), start=(cb == 0), stop=(cb == 31)),
                         reads=["sqb%d" % i, "ones"], writes=[PK[3]])
                P.op("dve", lambda e: e.tensor_scalar(out=mean[:], in0=pst[2][:], scalar1=1.0 / D, scalar2=None, op0=ALU.mult),
                     reads=[PK[2]], writes=["mean"])
                P.op("dve", lambda e: e.tensor_tensor(out=tmp[0][:], in0=mean[:], in1=mean[:], op=ALU.mult), reads=["mean"], writes=["tmp0"])
                P.op("dve", lambda e: e.scalar_tensor_tensor(out=rstd[:], in0=pst[3][:], scalar=1.0 / D, in1=tmp[0][:],
                                                             op0=ALU.mult, op1=ALU.subtract), reads=[PK[3], "tmp0"], writes=["rstd"])
                rsqrt_inplace(lambda rstd=rstd: rstd[:], "rstd", add_eps=True)
                for cb in range(32):
                    i = cb % 2
                    eng = "dve" if cb % 2 == 0 else "pool"
                    P.op(eng, lambda e, cb=cb, i=i: e.tensor_tensor(out=tmp[i][:], in0=yT[:, cb, :], in1=mean[:], op=ALU.subtract),
                         reads=["yT%d" % cb, "mean"], writes=["tmp%d" % i])
                    P.op(eng, lambda e, i=i: e.tensor_tensor(out=tmp[i][:], in0=tmp[i][:], in1=rstd[:], op=ALU.mult),
                         reads=["tmp%d" % i, "rstd"], writes=["tmp%d" % i])
                    P.op("act", lambda e, cb=cb, i=i: e.activation(out=R(yT[:, cb, :]), in_=tmp[i][:], func=AF.Identity,
                                                                  bias=l1[:, 1, cb:cb + 1], scale=l1[:, 0, cb:cb + 1]),
                         reads=["tmp%d" % i, "l1"], writes=["yT%d" % cb])
                for t4 in range(4):
                    tt = tg * 4 + t4
                    tsl = slice(t4 * 128, (t4 + 1) * 128)

                    def rt(e, tsl=tsl):
                        for cb in range(32):
                            e.matmul(pst[4][:, 0:NE], lhsT=R(yT[:, cb, tsl]), rhs=R(wr[:, cb, :]), start=(cb == 0), stop=False)
                        return e.matmul(pst[4][:, 0:NE], lhsT=R(ones[0:1, :]), rhs=R(brt[:]), start=False, stop=True)
                    P.op("pe", rt, reads=["yT%d" % cb for cb in range(32)] + wk("wr") + ["brt", "ones"], writes=[PK[4]])
                    P.op("dve", lambda e: e.tensor_copy(out=lg[:], in_=pst[4][:, 0:NE]), reads=[PK[4]], writes=["lg"])
                    P.op("dve", lambda e, tt=tt: e.max(out=mx8[:, tt, :], in_=lg[:]), reads=["lg"], writes=["mx8"])
                    P.op("dve", lambda e, tt=tt: e.max_index(out=mi8[:, tt, :], in_max=mx8[:, tt, :], in_values=lg[:]),
                         reads=["lg", "mx8"], writes=["mi8"])
                    P.op("dve", lambda e, tt=tt: e.tensor_scalar(out=R(maskall[:, tt, :]), in0=lg[:], scalar1=mx8[:, tt, 3:4],
                                                                 scalar2=None, op0=ALU.is_ge), reads=["lg", "mx8"], writes=["maskall"])
                    P.op("dve", lambda e, tt=tt: e.tensor_scalar(out=negm[:, tt:tt + 1], in0=mx8[:, tt, 0:1], scalar1=-1.0,
                                                                 scalar2=None, op0=ALU.mult), reads=["mx8"], writes=["negm"])
                    P.op("act", lambda e, tt=tt: e.activation(out=gates[:, tt, :], in_=mx8[:, tt, 0:4], func=AF.Exp,
                                                              bias=negm[:, tt:tt + 1], scale=1.0), reads=["mx8", "negm"], writes=["gates"])
                    P.op("dve", lambda e, tt=tt: e.reduce_sum(out=esum[:, tt:tt + 1], in_=gates[:, tt, :], axis=AX.X),
                         reads=["gates"], writes=["esum"])
                    P.op("dve", lambda e, tt=tt: e.reciprocal(out=esum[:, tt:tt + 1], in_=esum[:, tt:tt + 1]),
                         reads=["esum"], writes=["esum"])
                    P.op("dve", lambda e, tt=tt: e.tensor_scalar(out=gates[:, tt, :], in0=gates[:, tt, :], scalar1=esum[:, tt:tt + 1],
                                                                 scalar2=None, op0=ALU.mult), reads=["gates", "esum"], writes=["gates"])
                    P.op("dve", lambda e, tt=tt: e.tensor_copy(out=idxf[:, tt, :], in_=mi8[:, tt, 0:4]), reads=["mi8"], writes=["idxf"])
                    xs = x1s[0]
                    xk = "x1s0"
                    x1cnt += 1
                    for g4 in range(8):
                        pb = pst[5 + g4 % 2]
                        pbk = PK[5 + g4 % 2]

                        def trx(e, g4=g4, pb=pb, tsl=tsl):
                            for u in range(4):
                                ins = e.transpose(pb[:, u * 128:(u + 1) * 128], yT[:, g4 * 4 + u, tsl], ident[:])
                            return ins
                        P.op("pe", trx, reads=["yT%d" % (g4 * 4 + u) for u in range(4)] + ["ident"], writes=[pbk])
                        P.op("act", lambda e, g4=g4, pb=pb, xs=xs: e.copy(out=xs[:, g4 * 512:(g4 + 1) * 512], in_=pb[:]),
                             reads=[pbk], writes=[xk])
                    dma("act", x1d[tt * 128:(tt + 1) * 128, :], xs[:], [xk], ["x1d"], "c_" + xk)

            for tt in range(8):
                def pm(e, tt=tt):
                    for t2 in range(tt):
                        e.matmul(pst[4][:, 0:NE], lhsT=R(ones[:]), rhs=R(maskall[:, t2, :]), start=(t2 == 0), stop=False)
                    return e.matmul(pst[4][:, 0:NE], lhsT=R(ustr[:]), rhs=R(maskall[:, tt, :]), start=(tt == 0), stop=True)
                P.op("pe", pm, reads=["maskall", "ones", "ustr"], writes=[PK[4]])
                P.op("dve", lambda e, tt=tt: e.tensor_copy(out=posall[:, tt, :], in_=pst[4][:, 0:NE]), reads=[PK[4]], writes=["posall"])
                for k in range(4):
                    P.op("dve", lambda e, tt=tt, k=k: e.tensor_scalar(out=oh[:], in0=iot[:], scalar1=idxf[:, tt, k:k + 1], scalar2=None,
                                                                      op0=ALU.is_equal), reads=["iot", "idxf"], writes=["oh"])
                    P.op("dve", lambda e, tt=tt: e.tensor_tensor(out=oh[:], in0=oh[:], in1=posall[:, tt, :], op=ALU.mult),
                         reads=["oh", "posall"], writes=["oh"])
                    P.op("dve", lambda e, tt=tt, k=k: e.reduce_sum(out=possel[:, tt, k:k + 1], in_=oh[:], axis=AX.X),
                         reads=["oh"], writes=["possel"])
            P.op("dve", lambda e: e.scalar_tensor_tensor(out=destf[:], in0=idxf[:], scalar=float(SLOTS), in1=possel[:],
                                                         op0=ALU.mult, op1=ALU.add), reads=["idxf", "possel"], writes=["destf"])
            P.op("dve", lambda e: e.tensor_copy(out=desti[:], in_=destf[:]), reads=["destf"], writes=["desti"])
            for tt in range(8):
                for k in range(4):
                    P.op("pool", lambda e, tt=tt, k=k: e.indirect_dma_start(
                        out=btok, out_offset=bass.IndirectOffsetOnAxis(ap=desti[:, tt, k:k + 1], axis=0),
                        in_=tokid[:, tt:tt + 1], in_offset=None), reads=["desti", "tokid", "btok"], writes=["btok_s"], dsem="c_sc")
            P.barrier()
            P.emit()

        if stop_after == 'C':
            return nc
        with ExitStack() as ph:
            idxe = sbt(ph, "idxe", [128, 2], I32)
            xe = [sbt(ph, "xe%d" % i, [128, D]) for i in range(2)]
            xeT = sbt(ph, "xeT", [128, 32, SLOTS])
            NWT = 5
            wt = [sbt(ph, "wt%d" % i, [128, 8, 512]) for i in range(NWT)]
            actT = sbt(ph, "actT", [128, 16, SLOTS])
            gsb = sbt(ph, "gsb", [128, 4, SLOTS])
            sgb = [sbt(ph, "sgb%d" % i, [128, SLOTS]) for i in range(2)]
            uub = [sbt(ph, "uub%d" % i, [128, SLOTS]) for i in range(2)]
            ost = [[sbt(ph, "ost%d_%d" % (i, j), [128, 512]) for j in range(2)] for i in range(2)]
            bdn = sbt(ph, "bdn", [1, D])
            bgu = sbt(ph, "bgu", [128, NE, 32])
            dma("sp", bgu[:], b_gu, [], ["bgu"], "d_bgu")
            wc = 0
            for ex in range(n_experts_run):
                for s in range(2):
                    dma("sp", idxe[:, s:s + 1], btok[ex * SLOTS + s * 128: ex * SLOTS + (s + 1) * 128, :], [], ["idxe%d" % s], "d_idx%d" % s)
                    P.op("pool", lambda e, s=s: e.indirect_dma_start(
                        out=xe[s][:], out_offset=None, in_=x1d,
                        in_offset=bass.IndirectOffsetOnAxis(ap=idxe[:, s:s + 1], axis=0)),
                        reads=["idxe%d" % s], writes=["xe%d" % s], dsem="d_g%d" % s)
                dma("sp", R(bdn[:]), R(b_dn[ex:ex + 1, :]), [], ["bdn"], "d_bdn")
                for s in range(2):
                    for g4 in range(8):
                        pb = pst[6 + g4 % 2]
                        pbk = PK[6 + g4 % 2]

                        def trx(e, g4=g4, pb=pb, s=s):
                            for u in range(4):
                                ins = e.transpose(pb[:, u * 128:(u + 1) * 128], xe[s][:, (g4 * 4 + u) * 128:(g4 * 4 + u + 1) * 128], ident[:])
                            return ins
                        P.op("pe", trx, reads=["xe%d" % s, "ident"], writes=[pbk])
                        P.op("act", lambda e, g4=g4, pb=pb, s=s: e.copy(
                            out=R(xeT[:, g4 * 4:(g4 + 1) * 4, s * 128:(s + 1) * 128]),
                            in_=pb[:].rearrange("p (u t) -> p u t", u=4)), reads=[pbk], writes=["xeT"])
                for sbi, sb_ in enumerate([0, 4, 1, 5, 2, 6, 3, 7]):
                    is_up = sb_ >= 4
                    pg = (sbi % 2) * 2
                    for kg in range(4):
                        i = wc % NWT
                        wc += 1
                        dma_kc(wt[i], w_gu[ex, kg * 1024:(kg + 1) * 1024, sb_ * 512:(sb_ + 1) * 512]
                               .rearrange("(kc p) c -> p kc c", p=128), 8, 4, ["wt%d" % i], "d_wt%d" % i)

                        def mg(e, i=i, kg=kg, pg=pg):
                            for blk in range(4):
                                po = pst[blk][:, 0:256]
                                for kc in range(8):
                                    ins = e.matmul(po, lhsT=R(wt[i][:, kc, blk * 128:(blk + 1) * 128]), rhs=R(xeT[:, kg * 8 + kc, :]),
                                                   start=(kg == 0 and kc == 0), stop=(kg == 3 and kc == 7))
                            return ins
                        P.op("pe", mg, reads=wk("wt%d" % i, 8, 4) + ["xeT"], writes=[PK[0], PK[1], PK[2], PK[3]])
                    for blk in range(4):
                        po = pst[blk][:, 0:256]
                        pk = PK[blk]
                        cbi = sb_ * 4 + blk
                        bias_ap = bgu[:, ex, cbi:cbi + 1]
                        if not is_up:
                            P.op("dve", lambda e, po=po, blk=blk, bias_ap=bias_ap: e.tensor_scalar(
                                out=gsb[:, blk, :], in0=po, scalar1=bias_ap, scalar2=7.0, op0=ALU.add, op1=ALU.min),
                                reads=[pk, "bgu"], writes=["gsb%d" % blk])
                            j = blk % 2
                            P.op("act", lambda e, blk=blk, j=j: e.activation(out=sgb[j][:], in_=gsb[:, blk, :], func=AF.Sigmoid, scale=1.702),
                                 reads=["gsb%d" % blk], writes=["sgb%d" % j])
                            P.op("pool", lambda e, blk=blk, j=j: e.tensor_tensor(out=gsb[:, blk, :], in0=gsb[:, blk, :], in1=sgb[j][:], op=ALU.mult),
                                 reads=["gsb%d" % blk, "sgb%d" % j], writes=["gsb%d" % blk])
                        else:
                            j = blk % 2
                            P.op("dve", lambda e, po=po, j=j, bias_ap=bias_ap: e.tensor_scalar(
                                out=uub[j][:], in0=po, scalar1=bias_ap, scalar2=7.0, op0=ALU.add, op1=ALU.min),
                                reads=[pk, "bgu"], writes=["uub%d" % j])
                            P.op("dve", lambda e, j=j: e.tensor_scalar(out=uub[j][:], in0=uub[j][:], scalar1=-7.0, scalar2=1.0,
                                                                       op0=ALU.max, op1=ALU.add), reads=["uub%d" % j], writes=["uub%d" % j])
                            ai = (sb_ - 4) * 4 + blk
                            P.op("pool", lambda e, j=j, blk=blk, ai=ai: e.tensor_tensor(out=R(actT[:, ai, :]), in0=uub[j][:], in1=gsb[:, blk, :], op=ALU.mult),
                                 reads=["uub%d" % j, "gsb%d" % blk], writes=["actT"])
                for db in range(8):
                    for kg in range(2):
                        i = wc % NWT
                        wc += 1
                        dma_kc(wt[i], w_dn[ex, kg * 1024:(kg + 1) * 1024, db * 512:(db + 1) * 512]
                               .rearrange("(kc p) c -> p kc c", p=128), 8, 4, ["wt%d" % i], "d_wt%d" % i)

                        def md(e, i=i, kg=kg, db=db):
                            for s in range(2):
                                for kc in range(8):
                                    ins = e.matmul(pst[4 + s][:], lhsT=R(actT[:, kg * 8 + kc, s * 128:(s + 1) * 128]), rhs=R(wt[i][:, kc, :]),
                                                   start=(kg == 0 and kc == 0), stop=False)
                                if kg == 1:
                                    ins = e.matmul(pst[4 + s][:], lhsT=R(ones[0:1, :]), rhs=R(bdn[0:1, db * 512:(db + 1) * 512]),
                                                   start=False, stop=True)
                            return ins
                        P.op("pe", md, reads=wk("wt%d" % i, 8, 4) + ["actT", "bdn", "ones"], writes=[PK[4], PK[5]])
                    for s in range(2):
                        ob = ost[s][db % 2]
                        ok_ = "ost%d_%d" % (s, db % 2)
                        if s == 0:
                            P.op("act", lambda e, s=s, ob=ob: e.copy(out=ob[:], in_=pst[4 + s][:]), reads=[PK[4 + s]], writes=[ok_])
                        else:
                            P.op("dve", lambda e, s=s, ob=ob: e.tensor_copy(out=ob[:], in_=pst[4 + s][:]), reads=[PK[4 + s]], writes=[ok_])
                        dma("act", Ed[ex * SLOTS + s * 128: ex * SLOTS + (s + 1) * 128, db * 512:(db + 1) * 512], ob[:], [ok_], [], "d_" + ok_)
            P.barrier()
            P.emit()

        if stop_after == 'D':
            return nc
        with ExitStack() as ph:
            xa = sbt(ph, "xa", [128, D])
            gk = [sbt(ph, "gk%d" % i, [128, D]) for i in range(2)]
            gbc = sbt(ph, "gbc", [128, D])
            bbc = sbt(ph, "bbc", [128, D])
            sqt = sbt(ph, "sqt", [128, D])
            ot = sbt(ph, "ot", [128, D])
            st1 = sbt(ph, "st1", [128, 4])
            dma("sp", gbc[:], bass.AP(ln2p_h, 0, [[0, 128], [1, D]]), [], ["gbc"], "e_g")
            dma("sp", bbc[:], bass.AP(ln2p_h, D, [[0, 128], [1, D]]), [], ["bbc"], "e_b")
            gc = 0
            outs = []
            for tt in range(8):
                dma("sp", xa[:], x1d[tt * 128:(tt + 1) * 128, :], [], ["xa"], "e_x")
                P.op("dve", lambda e: e.tensor_scalar(out=xa[:], in0=xa[:], scalar1=ALPHA, scalar2=None, op0=ALU.mult),
                     reads=["xa"], writes=["xa"])
                for k in range(4):
                    i = gc % 2
                    gc += 1
                    P.op("pool", lambda e, i=i, tt=tt, k=k: e.indirect_dma_start(
                        out=gk[i][:], out_offset=None, in_=Ed,
                        in_offset=bass.IndirectOffsetOnAxis(ap=desti[:, tt, k:k + 1], axis=0)),
                        reads=["desti"], writes=["gk%d" % i], dsem="e_gk%d" % i)
                    P.op("dve", lambda e, i=i, tt=tt, k=k: e.scalar_tensor_tensor(
                        out=xa[:], in0=gk[i][:], scalar=gates[:, tt, k:k + 1], in1=xa[:], op0=ALU.mult, op1=ALU.add),
                        reads=["gk%d" % i, "xa", "gates"], writes=["xa"])
                P.op("dve", lambda e: e.reduce_sum(out=st1[:, 0:1], in_=xa[:], axis=AX.X), reads=["xa"], writes=["st0"])
                P.op("dve", lambda e: e.tensor_scalar(out=st1[:, 1:2], in0=st1[:, 0:1], scalar1=-1.0 / D, scalar2=None, op0=ALU.mult),
                     reads=["st0"], writes=["st1"])
                P.op("dve", lambda e: e.tensor_scalar(out=xa[:], in0=xa[:], scalar1=st1[:, 1:2], scalar2=None, op0=ALU.add),
                     reads=["xa", "st1"], writes=["xa"])
                P.op("act", lambda e: e.activation(out=sqt[:], in_=xa[:], func=AF.Square), reads=["xa"], writes=["sqt"])
                P.op("dve", lambda e: e.reduce_sum(out=st1[:, 2:3], in_=sqt[:], axis=AX.X), reads=["sqt"], writes=["st2"])
                P.op("dve", lambda e: e.tensor_scalar(out=st1[:, 3:4], in0=st1[:, 2:3], scalar1=1.0 / D, scalar2=EPS, op0=ALU.mult, op1=ALU.add),
                     reads=["st2"], writes=["st3"])
                rsqrt_inplace(lambda: st1[:, 3:4], "st3")
                P.op("dve", lambda e: e.scalar_tensor_tensor(out=ot[:], in0=xa[:], scalar=st1[:, 3:4], in1=gbc[:], op0=ALU.mult, op1=ALU.mult),
                     reads=["xa", "st3", "gbc"], writes=["ot"])
                P.op("pool", lambda e: e.tensor_tensor(out=ot[:], in0=ot[:], in1=bbc[:], op=ALU.add), reads=["ot", "bbc"], writes=["ot"])
                outs.append(dma("act", out[tt * 128:(tt + 1) * 128, :], ot[:], ["ot"], [], "e_out"))
            P.barrier()
            P.emit()
    return nc


def _t5_bucket_np(d):
    n = np.maximum(d, 0)
    nf = np.maximum(n, 1).astype(np.float32)
    large = 16 + (np.log(nf / np.float32(16)) / np.float32(math.log(128 / 16)) * np.float32(16)).astype(np.int32)
    large = np.minimum(large, 31)
    return np.where(n < 16, n, large)


def _tiles(xtok):
    T = xtok.shape[0]
    return np.ascontiguousarray(xtok.reshape(T // 128, 128, 32, 128).transpose(0, 3, 2, 1))


def make_in_maps(x, mem, rel_table, w_in, w_mem_kv, w_o, lambda_q1, lambda_k1, lambda_q2, lambda_k2,
                 subln_g, conv_w, conv_b, conv_ln_g, conv_ln_b, ln1_g, ln1_b, w_router, b_router,
                 w_gate_up, b_gate_up, w_down, b_down, ln2_g, ln2_b):
    f = lambda a: np.ascontiguousarray(np.asarray(a, dtype=np.float32))
    x = f(x); mem = f(mem); rel_table = f(rel_table)
    shared = {
        "w_in": f(w_in[0]), "w_mkv": f(w_mem_kv[0]),
        "w_o": f(np.asarray(w_o[0]).reshape(32, 128, 32, 128).transpose(2, 1, 0, 3).reshape(32, 128, 4096)),
        "lamv": f(np.concatenate([lambda_q1[0], lambda_k1[0], lambda_q2[0], lambda_k2[0]])[None, :]),
        "subln": f(np.asarray(subln_g[0]).reshape(2, 128).T),
        "convw": f(np.asarray(conv_w[0])[:, 0, :].reshape(31, 8, 128).transpose(2, 1, 0)),
        "convp": f(np.stack([np.asarray(conv_b[0]).reshape(8, 128).T, np.asarray(conv_ln_g[0]).reshape(8, 128).T,
                             np.asarray(conv_ln_b[0]).reshape(8, 128).T], axis=1)),
        "ln1p": f(np.stack([np.asarray(ln1_g[0]).reshape(32, 128).T, np.asarray(ln1_b[0]).reshape(32, 128).T], axis=1)),
        "w_rt": f(w_router[0]), "b_rt": f(np.asarray(b_router[0])[None, :]),
        "w_gu": f(w_gate_up[0][:NEXP_UPLOAD]), "b_gu": f(np.asarray(b_gate_up[0]).reshape(NE, 32, 128).transpose(2, 0, 1)),
        "w_dn": f(w_down[0][:NEXP_UPLOAD]), "b_dn": f(b_down[0]),
        "ln2p": f(np.stack([np.asarray(ln2_g[0]), np.asarray(ln2_b[0])], axis=0)),
    }
    ext = np.concatenate([rel_table, np.full((1, 8), NEG, np.float32)], axis=0)
    in_maps = []
    for c in range(8):
        b, h = c // 2, c % 2
        xb = x[b]
        own = xb[h * 1024:(h + 1) * 1024]
        xr = xb.reshape(16, 128, D)[:, ::-1, :].reshape(2048, D)
        halo = xb[h * 1024 - 32: h * 1024] if h == 1 else np.zeros((32, D), np.float32)
        dd = np.arange(3072) - 2047 + h * 1024
        bidx = np.where(dd < 0, 32, _t5_bucket_np(dd))
        gvec = np.ascontiguousarray(ext[bidx].T)
        m = dict(shared)
        m.update({
            "xkv": _tiles(xr), "xq": _tiles(own),
            "xh": np.ascontiguousarray(halo.reshape(32, 32, 128).transpose(2, 1, 0)),
            "xqT": np.ascontiguousarray(own.T), "memt": _tiles(mem[b]), "gvec": gvec,
        })
        in_maps.append(m)
    return in_maps


def kernel(**inputs):
    in_maps = make_in_maps(**inputs)
    nc = build()
    res = run_bass_kernel_spmd(nc, in_maps, core_ids=list(range(8)))
    out = np.empty((4, 2048, D), np.float32)
    for c in range(8):
        b, h = c // 2, c % 2
        out[b, h * 1024:(h + 1) * 1024] = res.results[c]["out"]
    return out
```

```python
import math
from contextlib import ExitStack
import numpy as np
import concourse.bass as bass
import concourse.mybir as mybir
from concourse.bass_utils import run_bass_kernel_spmd

F32 = mybir.dt.float32
F32R = mybir.dt.float32r
I32 = mybir.dt.int32
U32 = mybir.dt.uint32
ALU = mybir.AluOpType
AF = mybir.ActivationFunctionType
AX = mybir.AxisListType

D = 4096
NE = 32
SLOTS = 256
ALPHA = 2.0 ** 0.25
LAM_INIT = 0.2
EPS = 1e-5
NEG = -30000.0
NEXP_UPLOAD = NE


def R(ap):
    return ap.bitcast(F32R)


class Prog:
    ENGS = ("pe", "act", "dve", "pool", "sp")

    def __init__(self, nc, stack):
        self.nc = nc
        self.stack = stack
        self.q = {e: [] for e in self.ENGS}
        self.psem = {}
        for e in ("pe", "act", "dve", "pool"):
            self.psem[e] = stack.enter_context(nc.semaphore("prog_" + e))
        self.cnt = {e: 0 for e in self.ENGS}
        self.waited = {e: {} for e in self.ENGS}
        self.state = {}
        self.dma_sems = {}
        self.dma_cnt = {}

    def _st(self, key):
        if key not in self.state:
            self.state[key] = {"w": None, "r": []}
        return self.state[key]

    def _prune(self, eng, deps):
        need = {}
        for sem, val in deps:
            k = id(sem)
            if self.waited[eng].get(k, 0) >= val:
                continue
            if k not in need or need[k][1] < val:
                need[k] = (sem, val)
        for k, (sem, val) in need.items():
            self.waited[eng][k] = val
        return list(need.values())

    def op(self, eng, fn, reads=(), writes=(), dsem=None):
        writes = list(writes) + [r for r in reads if r.startswith("ps") and r[2:].isdigit() and r not in writes]
        deps = []
        own = self.psem.get(eng) if dsem is None else None
        for r in reads:
            st = self._st(r)
            if st["w"] is not None:
                deps.append(st["w"])
        for w in writes:
            st = self._st(w)
            if st["w"] is not None and st["w"][0] is not own:
                deps.append(st["w"])
            for t in st["r"]:
                if t[0] is not own:
                    deps.append(t)
        deps = self._prune(eng, deps)
        if dsem is not None:
            if dsem not in self.dma_sems:
                self.dma_sems[dsem] = self.stack.enter_context(self.nc.semaphore("d_" + dsem))
                self.dma_cnt[dsem] = 0
            self.dma_cnt[dsem] += 16
            tok = (self.dma_sems[dsem], self.dma_cnt[dsem])
            inc = 16
        else:
            self.cnt[eng] += 1
            tok = (self.psem[eng], self.cnt[eng])
            inc = 1
        self.q[eng].append((deps, fn, tok[0], inc))
        for r in reads:
            self._st(r)["r"].append(tok)
        for w in writes:
            self.state[w] = {"w": tok, "r": []}
        return tok

    def barrier(self):
        toks = [(self.psem[e], self.cnt[e]) for e in ("pe", "act", "dve", "pool") if self.cnt[e] > 0]
        toks += [(self.dma_sems[n], self.dma_cnt[n]) for n in self.dma_sems]
        for e in self.ENGS:
            deps = self._prune(e, [t for t in toks if t[0] is not self.psem.get(e)])
            if deps:
                self.q[e].append((deps, None, None, 0))
        self.state = {}

    def emit(self):
        nc = self.nc
        q = self.q

        def run(e, items):
            for deps, fn, sem, inc in items:
                for s, v in deps:
                    e.wait_ge(s, v)
                if fn is not None:
                    fn(e).then_inc(sem, inc)

        with nc.Block() as block:
            @block.tensor
            def _(e):
                run(e, q["pe"])

            @block.scalar
            def _(e):
                run(e, q["act"])

            @block.vector
            def _(e):
                run(e, q["dve"])

            @block.gpsimd
            def _(e):
                run(e, q["pool"])

            @block.sync
            def _(e):
                run(e, q["sp"])
        self.q = {e: [] for e in self.ENGS}


def build(dbg=False, n_experts_run=NE, stop_after=None, n_heads=8, do_conv=True, do_mem=True, head_stage=3):
    nc = bass.Bass("TRN2", target_bir_lowering=False)
    nc.dge_precook = False

    def din(name, shape, dt=F32):
        return nc.dram_tensor(name, list(shape), dt, kind="ExternalInput")

    xkv_t = din("xkv", [16, 128, 32, 128]).ap()
    xq_t = din("xq", [8, 128, 32, 128]).ap()
    xh_t = din("xh", [128, 32, 32]).ap()
    xqT = din("xqT", [D, 1024]).ap()
    mem_t = din("memt", [2, 128, 32, 128]).ap()
    gvec_h = din("gvec", [8, 3072])
    w_in = din("w_in", [D, 9216]).ap().rearrange("(kc p) c -> p kc c", p=128)
    w_mkv = din("w_mkv", [D, 2048]).ap().rearrange("(kc p) c -> p kc c", p=128)
    w_o = din("w_o", [32, 128, 32 * 128]).ap()
    lamv_h = din("lamv", [1, 512])
    subln = din("subln", [128, 2]).ap()
    convw = din("convw", [128, 8, 31]).ap()
    convp = din("convp", [128, 3, 8]).ap()
    ln1p = din("ln1p", [128, 2, 32]).ap()
    w_rt = din("w_rt", [D, NE]).ap().rearrange("(kc p) c -> p kc c", p=128)
    b_rt = din("b_rt", [1, NE]).ap()
    w_gu = din("w_gu", [n_experts_run, D, D]).ap()
    b_gu = din("b_gu", [128, NE, 32]).ap()
    w_dn = din("w_dn", [n_experts_run, 2048, D]).ap()
    b_dn = din("b_dn", [NE, D]).ap()
    ln2p_h = din("ln2p", [2, D])
    out = nc.dram_tensor("out", [1024, D], F32, kind="ExternalOutput").ap()
    kd = "ExternalOutput" if dbg else "Internal"
    mixT = nc.dram_tensor("mixT", [D, 1024], F32, kind=kd).ap()
    x1d = nc.dram_tensor("x1d", [1024, D], F32, kind=kd).ap()
    Ed = nc.dram_tensor("Ed", [NE * SLOTS, D], F32, kind="Internal").ap()
    btok = nc.dram_tensor("btok", [NE * SLOTS, 1], I32, kind=kd).ap()

    with ExitStack() as top:
        P = Prog(nc, top)
        sbt = lambda st, name, shape, dt=F32: st.enter_context(nc.sbuf_tensor(name, list(shape), dt))
        pst = [top.enter_context(nc.psum_tensor("psb%d" % i, [128, 512], F32)) for i in range(8)]
        PK = ["ps%d" % i for i in range(8)]

        ident = sbt(top, "ident", [128, 128])
        ones = sbt(top, "ones", [128, 128])
        ustr = sbt(top, "ustr", [128, 128])
        neglam = sbt(top, "neglam", [128, 1])
        g8 = sbt(top, "g8", [128, 2])
        gates = sbt(top, "gates", [128, 8, 4])
        desti = sbt(top, "desti", [128, 8, 4], I32)

        cz = sbt(top, "cz", [128, 128])
        P.op("pool", lambda e: e.memset(cz[:], 0.0), writes=["cz"])
        P.op("pool", lambda e: e.affine_select(out=cz[:], in_=cz[:], pattern=[[-1, 128]],
                                               compare_op=ALU.not_equal, fill=1.0, base=0, channel_multiplier=1),
             reads=["cz"], writes=["cz"])
        P.op("dve", lambda e: e.tensor_copy(out=R(ident[:]), in_=cz[:]), reads=["cz"], writes=["ident"])
        P.op("pool", lambda e: e.memset(cz[:], 1.0), reads=[], writes=["cz"])
        P.op("dve", lambda e: e.tensor_copy(out=R(ones[:]), in_=cz[:]), reads=["cz"], writes=["ones"])
        P.op("pool", lambda e: e.affine_select(out=cz[:], in_=cz[:], pattern=[[1, 128]],
                                               compare_op=ALU.is_gt, fill=0.0, base=0, channel_multiplier=-1),
             reads=["cz"], writes=["cz"])
        P.op("dve", lambda e: e.tensor_copy(out=R(ustr[:]), in_=cz[:]), reads=["cz"], writes=["ustr"])


        def rsqrt_inplace(ap_fn, key, add_eps=False):
            if add_eps:
                P.op("dve", lambda e: e.tensor_scalar(out=ap_fn(), in0=ap_fn(), scalar1=EPS, scalar2=None, op0=ALU.add),
                     reads=[key], writes=[key])
            P.op("act", lambda e: e.activation(out=ap_fn(), in_=ap_fn(), func=AF.Sqrt), reads=[key], writes=[key])
            P.op("dve", lambda e: e.reciprocal(out=ap_fn(), in_=ap_fn()), reads=[key], writes=[key])

        def dma(eng, out_ap, in_ap, reads, writes, sem):
            return P.op(eng, lambda e, o=out_ap, i=in_ap: e.dma_start(out=o, in_=i), reads=reads, writes=writes, dsem=sem)


        pending = []

        def dma_kc(out3, in3, nk, step, writes, sem, eng="sp"):
            for k0 in range(0, nk, step):
                args = ("sp" if eng == "defer" else eng, R(out3[:, k0:k0 + step, :]), R(in3[:, k0:k0 + step, :]), [],
                        ["%s.%d" % (writes[0], k0)], sem)
                if eng == "defer":
                    pending.append(args)
                else:
                    dma(*args)

        def flush_pending(n=None):
            k = len(pending) if n is None else min(n, len(pending))
            for _ in range(k):
                dma(*pending.pop(0))

        def wk(base, nk=32, step=4):
            return ["%s.%d" % (base, k0) for k0 in range(0, nk, step)]

        with ExitStack() as ph:
            wkv = sbt(ph, "wkv", [128, 32, 512])
            wq = sbt(ph, "wq", [128, 32, 256])
            NXB = 2
            xb = [sbt(ph, "xb%d" % i, [128, 32, 128]) for i in range(NXB)]
            big = sbt(ph, "big", [128, 10240])
            KT = big[:, 0:4096].rearrange("p (m t) -> p m t", m=2)
            V = big[:, 4096:8192].rearrange("p (m t) -> p m t", m=16)
            QT = big[:, 8192:10240].rearrange("p (m t) -> p m t", m=2)
            hT = big[:, 0:8448].rearrange("p (m t) -> p m t", m=8)
            acc = wq[:].rearrange("p a b -> p (a b)").rearrange("p (m t) -> p m t", m=8)
            ksb = [sbt(ph, "ksb%d" % i, [128, 256]) for i in range(2)]
            btt = sbt(ph, "btt", [128, 2, 512])
            bt = [btt[:, i, :] for i in range(2)]
            xhb = btt[:].rearrange("p a b -> p (a b)").rearrange("p (k t) -> p k t", k=32)
            pt = [sbt(ph, "pt%d" % i, [128, 512]) for i in range(2)]
            rec = sbt(ph, "rec", [128, 512])
            o1 = sbt(ph, "o1", [128, 2, 512])
            of = sbt(ph, "of", [128, 2, 512])
            sq = sbt(ph, "sq", [128, 2, 512])
            rstd = sbt(ph, "rstd", [128, 512])
            ao = [sbt(ph, "ao%d" % i, [128, 2, 512]) for i in range(2)]
            lv = of[:, 0, :]
            lpr = of[:, 1, 0:256]
            ls = sbt(ph, "ls", [128, 4])
            sub_sb = sbt(ph, "sub_sb", [128, 2])
            cw = sbt(ph, "cw", [128, 8, 31])
            cp = sbt(ph, "cp", [128, 3, 8])
            sig = [pt[i][:, 0:256] for i in range(2)]
            mean = rec
            tmp = o1[:, 0, :]

            dma("sp", lv[:], bass.AP(lamv_h, 0, [[0, 128], [1, 512]]), [], ["lv"], "misc")
            dma("sp", sub_sb[:], subln, [], ["sub_sb"], "misc2")
            dma("sp", cw[:], convw, [], ["cw"], "misc3")
            dma("sp", cp[:], convp, [], ["cp"], "misc4")
            P.op("dve", lambda e: e.tensor_tensor(out=lpr[:, 0:128], in0=lv[:, 0:128], in1=lv[:, 128:256], op=ALU.mult),
                 reads=["lv"], writes=["lpr0"])
            P.op("dve", lambda e: e.tensor_tensor(out=lpr[:, 128:256], in0=lv[:, 256:384], in1=lv[:, 384:512], op=ALU.mult),
                 reads=["lv"], writes=["lpr1"])
            P.op("dve", lambda e: e.reduce_sum(out=ls[:, 0:1], in_=lpr[:, 0:128], axis=AX.X), reads=["lpr0"], writes=["ls0"])
            P.op("dve", lambda e: e.reduce_sum(out=ls[:, 1:2], in_=lpr[:, 128:256], axis=AX.X), reads=["lpr1"], writes=["ls1"])
            P.op("act", lambda e: e.activation(out=ls[:, 2:4], in_=ls[:, 0:2], func=AF.Exp), reads=["ls0", "ls1"], writes=["ls2"])
            P.op("dve", lambda e: e.tensor_tensor(out=neglam[:], in0=ls[:, 3:4], in1=ls[:, 2:3], op=ALU.subtract),
                 reads=["ls2"], writes=["neglam"])
            P.op("dve", lambda e: e.tensor_scalar(out=neglam[:], in0=neglam[:], scalar1=-LAM_INIT, scalar2=None, op0=ALU.add),
                 reads=["neglam"], writes=["neglam"])
            P.op("dve", lambda e: e.tensor_scalar(out=g8[:], in0=sub_sb[:], scalar1=1.0 - LAM_INIT, scalar2=None, op0=ALU.mult),
                 reads=["sub_sb"], writes=["g8"])

            xcnt = [0]

            def proj(x_tiles, n_tiles, w_tile, wkey, N, evac, halo=False):
                wkeys = (wk("wkvA") + wk("wkvB")) if wkey == "wkv" else wk(wkey)
                for t in range(n_tiles):
                    i = xcnt[0] % NXB
                    ip = xcnt[0] % 2
                    xcnt[0] += 1
                    xkey = "xb%d" % i
                    pkey = PK[ip]
                    ps = pst[ip]
                    dma("sp", R(xb[i][:].rearrange("p k t -> p (k t)")), R(x_tiles[t].rearrange("p k t -> p (k t)")), [], [xkey], xkey)

                    def mm(e, i=i, ps=ps):
                        for kc in range(32):
                            ins = e.matmul(ps[:, 0:N], lhsT=R(xb[i][:, kc, :]), rhs=R(w_tile[:, kc, 0:N]),
                                           start=(kc == 0), stop=(kc == 31))
                        return ins
                    P.op("pe", mm, reads=[xkey] + wkeys, writes=[pkey])
                    evac(t, ps, pkey, 128)
                if halo:
                    dma("sp", R(xhb[:].rearrange("p k t -> p (k t)")), R(xh_t.rearrange("p k t -> p (k t)")), [], ["xhb"], "xhb")
                    ps = pst[2]

                    def mmh(e, ps=ps):
                        for kc in range(32):
                            ins = e.matmul(ps[0:32, 0:N], lhsT=R(xhb[:, kc, :]), rhs=R(w_tile[:, kc, 0:N]),
                                           start=(kc == 0), stop=(kc == 31))
                        return ins
                    P.op("pe", mmh, reads=["xhb"] + wkeys, writes=[PK[2]])
                    evac(-1, ps, PK[2], 32)

            tcnt = [0, 0]

            def transpose_to(src_sb, src_key, dst_ap_fn, dst_key, np_=128, scale_eng="dve"):
                ps = pst[3]

                def tr(e):
                    for m in range(2):
                        ins = e.transpose(ps[:, m * 128:m * 128 + np_], src_sb[0:np_, m * 128:(m + 1) * 128],
                                          ident[0:np_, 0:np_])
                    return ins
                P.op("pe", tr, reads=[src_key, "ident"], writes=[PK[3]])
                src = ps[:, 0:256].rearrange("p (m t) -> p m t", m=2)[:, :, 0:np_]
                P.op(scale_eng, lambda e: e.tensor_copy(out=R(dst_ap_fn()), in_=src), reads=[PK[3]], writes=[dst_key])

            def attention(hd_rows, n_dc, nkt_fn, bias_hd, nmaps, post):
                accb = [(6, 7, 2), (0, 1, 3)]
                for c in range(2):
                    nk = nkt_fn(c)

                    def emit_lg(j, c=c):
                        slots = []
                        bj = None
                        if bias_hd is not None:
                            bj = tcnt[1] % 2
                            tcnt[1] += 1
                            off = 1920 + 512 * c - 128 * j
                            dma("sp", R(bt[bj][:]), R(bass.AP(gvec_h, bias_hd * 3072 + off, [[1, 128], [1, 512]])),
                                [], ["bt%d" % bj], "bt%d" % bj)
                            flush_pending(1)
                        for m in range(nmaps):
                            jj = tcnt[0] % 2
                            tcnt[0] += 1
                            psl = pst[4 + jj]
                            plk = PK[4 + jj]
                            if bias_hd is not None:
                                def lg(e, psl=psl, m=m, j=j, bj=bj):
                                    e.matmul(psl[:], lhsT=R(KT[:, m, j * 128:(j + 1) * 128]),
                                             rhs=R(QT[:, m, c * 512:(c + 1) * 512]), start=True, stop=False)
                                    return e.matmul(psl[:], lhsT=R(ident[:]), rhs=R(bt[bj][:]), start=False, stop=True)
                                P.op("pe", lg, reads=["KT", "QT", "bt%d" % bj, "ident"], writes=[plk])
                            else:
                                def lg(e, psl=psl, j=j):
                                    for dc in range(n_dc):
                                        ins = e.matmul(psl[:], lhsT=R(KT[:, dc, j * 128:(j + 1) * 128]),
                                                       rhs=R(QT[:, dc, c * 512:(c + 1) * 512]),
                                                       start=(dc == 0), stop=(dc == n_dc - 1))
                                    return ins
                                P.op("pe", lg, reads=["KT", "QT"], writes=[plk])
                            P.op("act", lambda e, jj=jj, psl=psl: e.activation(out=R(pt[jj][:]), in_=psl[:], func=AF.Exp),
                                 reads=[plk], writes=["pt%d" % jj])
                            slots.append(jj)
                        return slots

                    def emit_pv(j, slots, nk=nk):
                        for m in range(nmaps):
                            jj = slots[m]
                            b0, b1, b2 = accb[m]

                            def pv(e, j=j, jj=jj, b0=b0, b1=b1, b2=b2):
                                e.matmul(pst[b0][:], lhsT=R(V[:, j, 0:128]), rhs=R(pt[jj][:]), start=(j == 0), stop=(j == nk - 1))
                                e.matmul(pst[b1][:], lhsT=R(V[:, j, 128:256]), rhs=R(pt[jj][:]), start=(j == 0), stop=(j == nk - 1))
                                return e.matmul(pst[b2][:], lhsT=R(ones[:]), rhs=R(pt[jj][:]), start=(j == 0), stop=(j == nk - 1))
                            P.op("pe", pv, reads=["V", "pt%d" % jj, "ones"], writes=[PK[b0], PK[b1], PK[b2]])

                    if nmaps == 2:
                        prev = emit_lg(0)
                        for j in range(nk):
                            emit_pv(j, prev)
                            prev = emit_lg(j + 1) if j + 1 < nk else None
                    else:
                        prev = emit_lg(0)
                        for j in range(nk):
                            nxt = emit_lg(j + 1) if j + 1 < nk else None
                            emit_pv(j, prev)
                            prev = nxt
                    for m in range(nmaps):
                        b0, b1, b2 = accb[m]
                        P.op("dve", lambda e, b2=b2: e.reciprocal(out=rec[:], in_=pst[b2][:]), reads=[PK[b2]], writes=["rec"])
                        dst = o1 if (nmaps == 2 and m == 0) else of
                        dk = "o1" if (nmaps == 2 and m == 0) else "of"
                        for dvc, bb in enumerate((b0, b1)):
                            P.op("dve", lambda e, dvc=dvc, dst=dst, bb=bb: e.tensor_tensor(
                                out=dst[:, dvc, :], in0=pst[bb][:], in1=rec[:], op=ALU.mult),
                                reads=[PK[bb], "rec"], writes=[dk + str(dvc)])
                    post(c)
                flush_pending()

            ocnt = [0]

            def store_mix(row0, c, src_tile, src_keys):
                for dvc in range(2):
                    dma("act", mixT[row0 + dvc * 128: row0 + (dvc + 1) * 128, c * 512:(c + 1) * 512],
                        src_tile[:, dvc, :], [src_keys[dvc]], [], "st_" + src_keys[dvc])

            def load_head_w(hd, eng="sp"):
                dma_kc(wkv[:, :, 0:256], w_in[:, :, 2048 + hd * 256: 2048 + (hd + 1) * 256], 32, 4, ["wkvA"], "wkv", eng)
                dma_kc(wkv[:, :, 256:512], w_in[:, :, 4096 + hd * 256: 4096 + (hd + 1) * 256], 32, 4, ["wkvB"], "wkv", eng)
                dma_kc(wq, w_in[:, :, hd * 256:(hd + 1) * 256], 32, 4, ["wq"], "wq", eng)

            def load_conv_w(ci, eng="sp"):
                dma_kc(wkv[:, :, 0:256], w_in[:, :, 6144 + ci * 256: 6144 + (ci + 1) * 256], 32, 4, ["wkvA"], "wkv", eng)
                dma_kc(wkv[:, :, 256:512], w_in[:, :, 7168 + ci * 256: 7168 + (ci + 1) * 256], 32, 4, ["wkvB"], "wkv", eng)

            def load_mem_w(hd, which, eng="sp"):
                if which in ("kv", "all"):
                    dma_kc(wkv[:, :, 0:256], w_mkv[:, :, hd * 256:(hd + 1) * 256], 32, 4, ["wkvA"], "wkv", eng)
                    dma_kc(wkv[:, :, 256:512], w_mkv[:, :, 1024 + hd * 256: 1024 + (hd + 1) * 256], 32, 4, ["wkvB"], "wkv", eng)
                if which in ("q", "all"):
                    dma_kc(wq, w_in[:, :, 8192 + hd * 256: 8192 + (hd + 1) * 256], 32, 4, ["wq"], "wq", eng)

            for hd in range(n_heads):
                if hd == 0:
                    load_head_w(0)

                def evac_kv(t, ps, pkey, np_):
                    i = t % 2
                    P.op("act", lambda e, i=i, ps=ps: e.copy(out=ksb[i][:], in_=ps[:, 0:256]), reads=[pkey], writes=["ksb%d" % i])
                    P.op("dve", lambda e, t=t, ps=ps: e.tensor_copy(out=R(V[:, t, :]), in_=ps[:, 256:512]), reads=[pkey], writes=["V"])
                    transpose_to(ksb[i], "ksb%d" % i, lambda t=t: KT[:, :, t * 128:(t + 1) * 128], "KT")
                proj(xkv_t, 16, wkv, "wkv", 512, evac_kv)

                def evac_q(t, ps, pkey, np_):
                    i = t % 2
                    P.op("act", lambda e, i=i, ps=ps: e.mul(out=ksb[i][:], in_=ps[:, 0:256], mul=128.0 ** -0.5),
                         reads=[pkey], writes=["ksb%d" % i])
                    transpose_to(ksb[i], "ksb%d" % i, lambda t=t: QT[:, :, t * 128:(t + 1) * 128], "QT")
                if head_stage >= 2:
                    proj(xq_t, 8, wq, "wq", 256, evac_q)

                def post_diff(c, hd=hd):
                    for dvc in range(2):
                        P.op("dve", lambda e, dvc=dvc: e.scalar_tensor_tensor(
                            out=of[:, dvc, :], in0=of[:, dvc, :], scalar=neglam[:, 0:1], in1=o1[:, dvc, :],
                            op0=ALU.mult, op1=ALU.add), reads=["of%d" % dvc, "o1%d" % dvc, "neglam"], writes=["of%d" % dvc])
                        P.op("act", lambda e, dvc=dvc: e.activation(out=R(sq[:, dvc, :]), in_=of[:, dvc, :], func=AF.Square),
                             reads=["of%d" % dvc], writes=["sq%d" % dvc])

                    def ssum(e):
                        e.matmul(pst[3][:], lhsT=R(ones[:]), rhs=R(sq[:, 0, :]), start=True, stop=False)
                        return e.matmul(pst[3][:], lhsT=R(ones[:]), rhs=R(sq[:, 1, :]), start=False, stop=True)
                    P.op("pe", ssum, reads=["sq0", "sq1", "ones"], writes=[PK[3]])
                    P.op("dve", lambda e: e.tensor_scalar(out=rstd[:], in0=pst[3][:], scalar1=1.0 / 256, scalar2=EPS,
                                                          op0=ALU.mult, op1=ALU.add), reads=[PK[3]], writes=["rstd"])
                    rsqrt_inplace(lambda: rstd[:], "rstd")
                    a = ao[ocnt[0] % 2]
                    ak = "ao%d_" % (ocnt[0] % 2)
                    ocnt[0] += 1
                    for dvc in range(2):
                        P.op("dve", lambda e, dvc=dvc, a=a: e.scalar_tensor_tensor(
                            out=a[:, dvc, :], in0=of[:, dvc, :], scalar=g8[:, dvc:dvc + 1], in1=rstd[:],
                            op0=ALU.mult, op1=ALU.mult), reads=["of%d" % dvc, "g8", "rstd"], writes=[ak + str(dvc)])
                    store_mix(hd * 256, c, a, [ak + "0", ak + "1"])

                if hd + 1 < n_heads:
                    load_head_w(hd + 1, "defer")
                elif do_conv:
                    load_conv_w(0, "defer")
                if head_stage >= 3:
                    attention(hd * 256, 1, lambda c: 12 + 4 * c, hd, 2, post_diff)

            P.barrier()
            for ci in range(4 if do_conv else 0):
                if ci > 0 or n_heads == 0:
                    load_conv_w(ci)

                def evac_glu(t, ps, pkey, np_, ci=ci):
                    i = (t + 2) % 2
                    P.op("act", lambda e, i=i, ps=ps: e.activation(out=R(sig[i][0:np_, :]), in_=ps[0:np_, 256:512], func=AF.Sigmoid),
                         reads=[pkey], writes=["pt%d" % i])
                    P.op("dve", lambda e, i=i, ps=ps: e.tensor_tensor(out=ksb[i][0:np_, :], in0=ps[0:np_, 0:256], in1=sig[i][0:np_, :],
                                                                     op=ALU.mult), reads=[pkey, "pt%d" % i], writes=["ksb%d" % i])
                    if t >= 0:
                        dst = lambda t=t: hT[:, 2 * ci:2 * ci + 2, 32 + t * 128: 32 + (t + 1) * 128]
                    else:
                        dst = lambda: hT[:, 2 * ci:2 * ci + 2, 0:32]
                    transpose_to(ksb[i], "ksb%d" % i, dst, "hT%d" % ci, np_=np_)
                proj(xq_t, 8, wkv, "wkv", 512, evac_glu, halo=True)

            if do_conv and do_mem:
                load_mem_w(0, "kv")
            for ch in range(8 if do_conv else 0):
                eng = "dve"
                hk = "hT%d" % (ch // 2)
                akey = "acc%d" % ch
                P.op(eng, lambda e, ch=ch: e.tensor_scalar(out=R(acc[:, ch, :]), in0=hT[:, ch, 2:1026], scalar1=cw[:, ch, 0:1],
                                                           scalar2=None, op0=ALU.mult), reads=[hk, "cw"], writes=[akey])
                for j in range(1, 31):
                    P.op(eng, lambda e, ch=ch, j=j: e.scalar_tensor_tensor(
                        out=R(acc[:, ch, :]), in0=hT[:, ch, 2 + j:1026 + j], scalar=cw[:, ch, j:j + 1], in1=acc[:, ch, :],
                        op0=ALU.mult, op1=ALU.add), reads=[hk, "cw", akey], writes=[akey])
                P.op(eng, lambda e, ch=ch: e.tensor_scalar(out=R(acc[:, ch, :]), in0=acc[:, ch, :], scalar1=cp[:, 0, ch:ch + 1],
                                                           scalar2=None, op0=ALU.add), reads=[akey, "cp"], writes=[akey])
            for c in range(2 if do_conv else 0):
                cs = slice(c * 512, (c + 1) * 512)
                for ch in range(8):
                    P.op("act", lambda e, ch=ch, cs=cs: e.activation(out=R(sq[:, ch % 2, :]), in_=acc[:, ch, cs], func=AF.Square),
                         reads=["acc%d" % ch], writes=["sq%d" % (ch % 2)])
                    P.op("pe", lambda e, ch=ch, cs=cs: e.matmul(pst[2][:], lhsT=R(ones[:]), rhs=R(acc[:, ch, cs]),
                                                                start=(ch == 0), stop=(ch == 7)),
                         reads=["acc%d" % ch, "ones"], writes=[PK[2]])
                    P.op("pe", lambda e, ch=ch: e.matmul(pst[3][:], lhsT=R(ones[:]), rhs=R(sq[:, ch % 2, :]),
                                                         start=(ch == 0), stop=(ch == 7)),
                         reads=["sq%d" % (ch % 2), "ones"], writes=[PK[3]])
                P.op("dve", lambda e: e.tensor_scalar(out=mean[:], in0=pst[2][:], scalar1=1.0 / 1024, scalar2=None, op0=ALU.mult),
                     reads=[PK[2]], writes=["mean"])
                P.op("dve", lambda e: e.tensor_tensor(out=tmp[:], in0=mean[:], in1=mean[:], op=ALU.mult), reads=["mean"], writes=["tmp"])
                P.op("dve", lambda e: e.scalar_tensor_tensor(out=rstd[:], in0=pst[3][:], scalar=1.0 / 1024, in1=tmp[:],
                                                             op0=ALU.mult, op1=ALU.subtract), reads=[PK[3], "tmp"], writes=["rstd"])
                rsqrt_inplace(lambda rstd=rstd: rstd[:], "rstd", add_eps=True)
                for ch in range(8):
                    a = ao[ocnt[0] % 2]
                    ak = "ao%d_" % (ocnt[0] % 2)
                    if ch % 2 == 1:
                        ocnt[0] += 1
                    d2 = ch % 2
                    P.op("dve", lambda e, ch=ch, cs=cs: e.tensor_tensor(out=tmp[:], in0=acc[:, ch, cs], in1=mean[:], op=ALU.subtract),
                         reads=["acc%d" % ch, "mean"], writes=["tmp"])
                    P.op("dve", lambda e: e.tensor_tensor(out=tmp[:], in0=tmp[:], in1=rstd[:], op=ALU.mult),
                         reads=["tmp", "rstd"], writes=["tmp"])
                    P.op("act", lambda e, ch=ch, a=a, d2=d2: e.activation(out=a[:, d2, :], in_=tmp[:], func=AF.Silu,
                                                                         bias=cp[:, 2, ch:ch + 1], scale=cp[:, 1, ch:ch + 1]),
                         reads=["tmp", "cp"], writes=[ak + str(d2)])
                    dma("act", mixT[2048 + ch * 128: 2048 + (ch + 1) * 128, cs], a[:, d2, :], [ak + str(d2)], [], "st_" + ak + str(d2))

            P.barrier()
            for hd in range(4 if do_mem else 0):
                if hd == 0:
                    load_mem_w(0, "q" if do_conv else "all")

                def evac_mkv(t, ps, pkey, np_):
                    i = t % 2
                    P.op("act", lambda e, i=i, ps=ps: e.copy(out=ksb[i][:], in_=ps[:, 0:256]), reads=[pkey], writes=["ksb%d" % i])
                    P.op("dve", lambda e, t=t, ps=ps: e.tensor_copy(out=R(V[:, t, :]), in_=ps[:, 256:512]), reads=[pkey], writes=["V"])
                    transpose_to(ksb[i], "ksb%d" % i, lambda t=t: KT[:, :, t * 128:(t + 1) * 128], "KT")
                proj(mem_t, 2, wkv, "wkv", 512, evac_mkv)

                def evac_mq(t, ps, pkey, np_):
                    i = t % 2
                    P.op("act", lambda e, i=i, ps=ps: e.mul(out=ksb[i][:], in_=ps[:, 0:256], mul=256.0 ** -0.5),
                         reads=[pkey], writes=["ksb%d" % i])
                    transpose_to(ksb[i], "ksb%d" % i, lambda t=t: QT[:, :, t * 128:(t + 1) * 128], "QT")
                proj(xq_t, 8, wq, "wq", 256, evac_mq)

                def post_mem(c, hd=hd):
                    a = ao[ocnt[0] % 2]
                    ak = "ao%d_" % (ocnt[0] % 2)
                    ocnt[0] += 1
                    for dvc in range(2):
                        P.op("act", lambda e, dvc=dvc, a=a: e.copy(out=a[:, dvc, :], in_=of[:, dvc, :]),
                             reads=["of%d" % dvc], writes=[ak + str(dvc)])
                    store_mix(3072 + hd * 256, c, a, [ak + "0", ak + "1"])
                if hd + 1 < 4:
                    load_mem_w(hd + 1, "all")
                attention(0, 2, lambda c: 2, None, 1, post_mem)

            P.barrier()
            P.emit()

        if stop_after == 'A':
            return nc
        with ExitStack() as ph:
            mix = sbt(ph, "mix", [128, 32, 512])
            yT = sbt(ph, "yT", [128, 32, 512])
            wo = [sbt(ph, "wo%d" % i, [128, 32, 128]) for i in range(2)]
            xt = [sbt(ph, "xt%d" % i, [128, 512]) for i in range(2)]
            sqb = [sbt(ph, "sqb%d" % i, [128, 512]) for i in range(2)]
            mean = sbt(ph, "mean1", [128, 512])
            rstd = sbt(ph, "rstd1", [128, 512])
            tmp = [sbt(ph, "tmp1_%d" % i, [128, 512]) for i in range(2)]
            l1 = sbt(ph, "l1", [128, 2, 32])
            wr = sbt(ph, "wr", [128, 32, NE])
            brt = sbt(ph, "brt", [1, NE])
            x1s = [sbt(ph, "x1s0", [128, D])]
            lg = sbt(ph, "lg", [128, NE])
            mx8 = sbt(ph, "mx8", [128, 8, 8])
            mi8 = sbt(ph, "mi8", [128, 8, 8], U32)
            idxf = sbt(ph, "idxf", [128, 8, 4])
            negm = sbt(ph, "negm", [128, 8])
            esum = sbt(ph, "esum", [128, 8])
            maskall = sbt(ph, "maskall", [128, 8, NE])
            posall = sbt(ph, "posall", [128, 8, NE])
            iot = sbt(ph, "iot", [128, NE])
            oh = sbt(ph, "oh", [128, NE])
            possel = sbt(ph, "possel", [128, 8, 4])
            destf = sbt(ph, "destf", [128, 8, 4])
            tokid = sbt(ph, "tokid", [128, 8], I32)
            zt = sbt(ph, "zt", [128, 64], I32)

            dma("sp", l1[:], ln1p, [], ["l1"], "c_l1")
            dma_kc(wr, w_rt, 32, 4, ["wr"], "c_wr")
            dma("sp", R(brt[:]), R(b_rt), [], ["brt"], "c_brt")
            P.op("pool", lambda e: e.iota(iot[:], pattern=[[1, NE]], base=0, channel_multiplier=0,
                                           allow_small_or_imprecise_dtypes=True), writes=["iot"])
            P.op("pool", lambda e: e.iota(tokid[:], pattern=[[128, 8]], base=0, channel_multiplier=1), writes=["tokid"])
            P.op("pool", lambda e: e.memset(zt[:], 0), writes=["zt"])
            dma("sp", btok.rearrange("(p f) o -> p (f o)", p=128), zt[:], ["zt"], ["btok"], "c_bt")

            wcnt = 0
            x1cnt = 0
            for tg in range(2):
                ts_ = slice(tg * 512, (tg + 1) * 512)
                for q4 in range(8):
                    dma("sp", R(mix[:, q4 * 4:(q4 + 1) * 4, :]),
                        R(mixT[q4 * 512:(q4 + 1) * 512, ts_].rearrange("(kc p) t -> p kc t", p=128)),
                        [], ["mix.%d" % q4], "c_mix")
                for cb in range(32):
                    i = wcnt % 2
                    wcnt += 1
                    dma("sp", R(wo[i][:].rearrange("p k c -> p (k c)")), R(w_o[cb]), [], ["wo%d" % i], "c_wo%d" % i)
                    dma("sp", xt[i][:], xqT[cb * 128:(cb + 1) * 128, ts_], [], ["xt%d" % i], "c_xt%d" % i)
                    ps = pst[i]

                    def mmo(e, i=i, ps=ps):
                        for kc in range(32):
                            ins = e.matmul(ps[:], lhsT=R(wo[i][:, kc, :]), rhs=R(mix[:, kc, :]), start=(kc == 0), stop=(kc == 31))
                        return ins
                    P.op("pe", mmo, reads=["wo%d" % i] + ["mix.%d" % q for q in range(8)], writes=[PK[i]])
                    P.op("dve", lambda e, i=i, ps=ps, cb=cb: e.scalar_tensor_tensor(
                        out=R(yT[:, cb, :]), in0=xt[i][:], scalar=ALPHA, in1=ps[:], op0=ALU.mult, op1=ALU.add),
                        reads=["xt%d" % i, PK[i]], writes=["yT%d" % cb])
                    P.op("act", lambda e, i=i, cb=cb: e.activation(out=R(sqb[i][:]), in_=yT[:, cb, :], func=AF.Square),
                         reads=["yT%d" % cb], writes=["sqb%d" % i])
                    P.op("pe", lambda e, cb=cb: e.matmul(pst[2][:], lhsT=R(ones[:]), rhs=R(yT[:, cb, :]), start=(cb == 0), stop=(cb == 31)),
                         reads=["yT%d" % cb, "ones"], writes=[PK[2]])
                    P.op("pe", lambda e, cb=cb, i=i: e.matmul(pst[3][:], lhsT=R(ones[:]), rhs=R(sqb[i][:]), start=(cb == 0), stop=(cb == 31)),
                         reads=["sqb%d" % i, "ones"], writes=[PK[3]])
                P.op("dve", lambda e: e.tensor_scalar(out=mean[:], in0=pst[2][:], scalar1=1.0 / D, scalar2=None, op0=ALU.mult),
                     reads=[PK[2]], writes=["mean"])
                P.op("dve", lambda e: e.tensor_tensor(out=tmp[0][:], in0=mean[:], in1=mean[:], op=ALU.mult), reads=["mean"], writes=["tmp0"])
                P.op("dve", lambda e: e.scalar_tensor_tensor(out=rstd[:], in0=pst[3][:], scalar=1.0 / D, in1=tmp[0][:],
                                                             op0=ALU.mult, op1=ALU.subtract), reads=[PK[3], "tmp0"], writes=["rstd"])
                rsqrt_inplace(lambda rstd=rstd: rstd[:], "rstd", add_eps=True)
                for cb in range(32):
                    i = cb % 2
                    eng = "dve" if cb % 2 == 0 else "pool"
                    P.op(eng, lambda e, cb=cb, i=i: e.tensor_tensor(out=tmp[i][:], in0=yT[:, cb, :], in1=mean[:], op=ALU.subtract),
                         reads=["yT%d" % cb, "mean"], writes=["tmp%d" % i])
                    P.op(eng, lambda e, i=i: e.tensor_tensor(out=tmp[i][:], in0=tmp[i][:], in1=rstd[:], op=ALU.mult),
                         reads=["tmp%d" % i, "rstd"], writes=["tmp%d" % i])
                    P.op("act", lambda e, cb=cb, i=i: e.activation(out=R(yT[:, cb, :]), in_=tmp[i][:], func=AF.Identity,
                                                                  bias=l1[:, 1, cb:cb + 1], scale=l1[:, 0, cb:cb + 1]),
                         reads=["tmp%d" % i, "l1"], writes=["yT%d" % cb])
                for t4 in range(4):
                    tt = tg * 4 + t4
                    tsl = slice(t4 * 128, (t4 + 1) * 128)

                    def rt(e, tsl=tsl):
                        for cb in range(32):
                            e.matmul(pst[4][:, 0:NE], lhsT=R(yT[:, cb, tsl]), rhs=R(wr[:, cb, :]), start=(cb == 0), stop=False)
                        return e.matmul(pst[4][:, 0:NE], lhsT=R(ones[0:1, :]), rhs=R(brt[:]), start=False, stop=True)
                    P.op("pe", rt, reads=["yT%d" % cb for cb in range(32)] + wk("wr") + ["brt", "ones"], writes=[PK[4]])
                    P.op("dve", lambda e: e.tensor_copy(out=lg[:], in_=pst[4][:, 0:NE]), reads=[PK[4]], writes=["lg"])
                    P.op("dve", lambda e, tt=tt: e.max(out=mx8[:, tt, :], in_=lg[:]), reads=["lg"], writes=["mx8"])
                    P.op("dve", lambda e, tt=tt: e.max_index(out=mi8[:, tt, :], in_max=mx8[:, tt, :], in_values=lg[:]),
                         reads=["lg", "mx8"], writes=["mi8"])
                    P.op("dve", lambda e, tt=tt: e.tensor_scalar(out=R(maskall[:, tt, :]), in0=lg[:], scalar1=mx8[:, tt, 3:4],
                                                                 scalar2=None, op0=ALU.is_ge), reads=["lg", "mx8"], writes=["maskall"])
                    P.op("dve", lambda e, tt=tt: e.tensor_scalar(out=negm[:, tt:tt + 1], in0=mx8[:, tt, 0:1], scalar1=-1.0,
                                                                 scalar2=None, op0=ALU.mult), reads=["mx8"], writes=["negm"])
                    P.op("act", lambda e, tt=tt: e.activation(out=gates[:, tt, :], in_=mx8[:, tt, 0:4], func=AF.Exp,
                                                              bias=negm[:, tt:tt + 1], scale=1.0), reads=["mx8", "negm"], writes=["gates"])
                    P.op("dve", lambda e, tt=tt: e.reduce_sum(out=esum[:, tt:tt + 1], in_=gates[:, tt, :], axis=AX.X),
                         reads=["gates"], writes=["esum"])
                    P.op("dve", lambda e, tt=tt: e.reciprocal(out=esum[:, tt:tt + 1], in_=esum[:, tt:tt + 1]),
                         reads=["esum"], writes=["esum"])
                    P.op("dve", lambda e, tt=tt: e.tensor_scalar(out=gates[:, tt, :], in0=gates[:, tt, :], scalar1=esum[:, tt:tt + 1],
                                                                 scalar2=None, op0=ALU.mult), reads=["gates", "esum"], writes=["gates"])
                    P.op("dve", lambda e, tt=tt: e.tensor_copy(out=idxf[:, tt, :], in_=mi8[:, tt, 0:4]), reads=["mi8"], writes=["idxf"])
                    xs = x1s[0]
                    xk = "x1s0"
                    x1cnt += 1
                    for g4 in range(8):
                        pb = pst[5 + g4 % 2]
                        pbk = PK[5 + g4 % 2]

                        def trx(e, g4=g4, pb=pb, tsl=tsl):
                            for u in range(4):
                                ins = e.transpose(pb[:, u * 128:(u + 1) * 128], yT[:, g4 * 4 + u, tsl], ident[:])
                            return ins
                        P.op("pe", trx, reads=["yT%d" % (g4 * 4 + u) for u in range(4)] + ["ident"], writes=[pbk])
                        P.op("act", lambda e, g4=g4, pb=pb, xs=xs: e.copy(out=xs[:, g4 * 512:(g4 + 1) * 512], in_=pb[:]),
                             reads=[pbk], writes=[xk])
                    dma("act", x1d[tt * 128:(tt + 1) * 128, :], xs[:], [xk], ["x1d"], "c_" + xk)

            for tt in range(8):
                def pm(e, tt=tt):
                    for t2 in range(tt):
                        e.matmul(pst[4][:, 0:NE], lhsT=R(ones[:]), rhs=R(maskall[:, t2, :]), start=(t2 == 0), stop=False)
                    return e.matmul(pst[4][:, 0:NE], lhsT=R(ustr[:]), rhs=R(maskall[:, tt, :]), start=(tt == 0), stop=True)
                P.op("pe", pm, reads=["maskall", "ones", "ustr"], writes=[PK[4]])
                P.op("dve", lambda e, tt=tt: e.tensor_copy(out=posall[:, tt, :], in_=pst[4][:, 0:NE]), reads=[PK[4]], writes=["posall"])
                for k in range(4):
                    P.op("dve", lambda e, tt=tt, k=k: e.tensor_scalar(out=oh[:], in0=iot[:], scalar1=idxf[:, tt, k:k + 1], scalar2=None,
                                                                      op0=ALU.is_equal), reads=["iot", "idxf"], writes=["oh"])
                    P.op("dve", lambda e, tt=tt: e.tensor_tensor(out=oh[:], in0=oh[:], in1=posall[:, tt, :], op=ALU.mult),
                         reads=["oh", "posall"], writes=["oh"])
                    P.op("dve", lambda e, tt=tt, k=k: e.reduce_sum(out=possel[:, tt, k:k + 1], in_=oh[:], axis=AX.X),
                         reads=["oh"], writes=["possel"])
            P.op("dve", lambda e: e.scalar_tensor_tensor(out=destf[:], in0=idxf[:], scalar=float(SLOTS), in1=possel[:],
                                                         op0=ALU.mult, op1=ALU.add), reads=["idxf", "possel"], writes=["destf"])
            P.op("dve", lambda e: e.tensor_copy(out=desti[:], in_=destf[:]), reads=["destf"], writes=["desti"])
            for tt in range(8):
                for k in range(4):
                    P.op("pool", lambda e, tt=tt, k=k: e.indirect_dma_start(
                        out=btok, out_offset=bass.IndirectOffsetOnAxis(ap=desti[:, tt, k:k + 1], axis=0),
                        in_=tokid[:, tt:tt + 1], in_offset=None), reads=["desti", "tokid", "btok"], writes=["btok_s"], dsem="c_sc")
            P.barrier()
            P.emit()

        if stop_after == 'C':
            return nc
        with ExitStack() as ph:
            idxe = sbt(ph, "idxe", [128, 2], I32)
            xe = [sbt(ph, "xe%d" % i, [128, D]) for i in range(2)]
            xeT = sbt(ph, "xeT", [128, 32, SLOTS])
            NWT = 5
            wt = [sbt(ph, "wt%d" % i, [128, 8, 512]) for i in range(NWT)]
            actT = sbt(ph, "actT", [128, 16, SLOTS])
            gsb = sbt(ph, "gsb", [128, 4, SLOTS])
            sgb = [sbt(ph, "sgb%d" % i, [128, SLOTS]) for i in range(2)]
            uub = [sbt(ph, "uub%d" % i, [128, SLOTS]) for i in range(2)]
            ost = [[sbt(ph, "ost%d_%d" % (i, j), [128, 512]) for j in range(2)] for i in range(2)]
            bdn = sbt(ph, "bdn", [1, D])
            bgu = sbt(ph, "bgu", [128, NE, 32])
            dma("sp", bgu[:], b_gu, [], ["bgu"], "d_bgu")
            wc = 0
            for ex in range(n_experts_run):
                for s in range(2):
                    dma("sp", idxe[:, s:s + 1], btok[ex * SLOTS + s * 128: ex * SLOTS + (s + 1) * 128, :], [], ["idxe%d" % s], "d_idx%d" % s)
                    P.op("pool", lambda e, s=s: e.indirect_dma_start(
                        out=xe[s][:], out_offset=None, in_=x1d,
                        in_offset=bass.IndirectOffsetOnAxis(ap=idxe[:, s:s + 1], axis=0)),
                        reads=["idxe%d" % s], writes=["xe%d" % s], dsem="d_g%d" % s)
                dma("sp", R(bdn[:]), R(b_dn[ex:ex + 1, :]), [], ["bdn"], "d_bdn")
                for s in range(2):
                    for g4 in range(8):
                        pb = pst[6 + g4 % 2]
                        pbk = PK[6 + g4 % 2]

                        def trx(e, g4=g4, pb=pb, s=s):
                            for u in range(4):
                                ins = e.transpose(pb[:, u * 128:(u + 1) * 128], xe[s][:, (g4 * 4 + u) * 128:(g4 * 4 + u + 1) * 128], ident[:])
                            return ins
                        P.op("pe", trx, reads=["xe%d" % s, "ident"], writes=[pbk])
                        P.op("act", lambda e, g4=g4, pb=pb, s=s: e.copy(
                            out=R(xeT[:, g4 * 4:(g4 + 1) * 4, s * 128:(s + 1) * 128]),
                            in_=pb[:].rearrange("p (u t) -> p u t", u=4)), reads=[pbk], writes=["xeT"])
                for sbi, sb_ in enumerate([0, 4, 1, 5, 2, 6, 3, 7]):
                    is_up = sb_ >= 4
                    pg = (sbi % 2) * 2
                    for kg in range(4):
                        i = wc % NWT
                        wc += 1
                        dma_kc(wt[i], w_gu[ex, kg * 1024:(kg + 1) * 1024, sb_ * 512:(sb_ + 1) * 512]
                               .rearrange("(kc p) c -> p kc c", p=128), 8, 4, ["wt%d" % i], "d_wt%d" % i)

                        def mg(e, i=i, kg=kg, pg=pg):
                            for blk in range(4):
                                po = pst[blk][:, 0:256]
                                for kc in range(8):
                                    ins = e.matmul(po, lhsT=R(wt[i][:, kc, blk * 128:(blk + 1) * 128]), rhs=R(xeT[:, kg * 8 + kc, :]),
                                                   start=(kg == 0 and kc == 0), stop=(kg == 3 and kc == 7))
                            return ins
                        P.op("pe", mg, reads=wk("wt%d" % i, 8, 4) + ["xeT"], writes=[PK[0], PK[1], PK[2], PK[3]])
                    for blk in range(4):
                        po = pst[blk][:, 0:256]
                        pk = PK[blk]
                        cbi = sb_ * 4 + blk
                        bias_ap = bgu[:, ex, cbi:cbi + 1]
                        if not is_up:
                            P.op("dve", lambda e, po=po, blk=blk, bias_ap=bias_ap: e.tensor_scalar(
                                out=gsb[:, blk, :], in0=po, scalar1=bias_ap, scalar2=7.0, op0=ALU.add, op1=ALU.min),
                                reads=[pk, "bgu"], writes=["gsb%d" % blk])
                            j = blk % 2
                            P.op("act", lambda e, blk=blk, j=j: e.activation(out=sgb[j][:], in_=gsb[:, blk, :], func=AF.Sigmoid, scale=1.702),
                                 reads=["gsb%d" % blk], writes=["sgb%d" % j])
                            P.op("pool", lambda e, blk=blk, j=j: e.tensor_tensor(out=gsb[:, blk, :], in0=gsb[:, blk, :], in1=sgb[j][:], op=ALU.mult),
                                 reads=["gsb%d" % blk, "sgb%d" % j], writes=["gsb%d" % blk])
                        else:
                            j = blk % 2
                            P.op("dve", lambda e, po=po, j=j, bias_ap=bias_ap: e.tensor_scalar(
                                out=uub[j][:], in0=po, scalar1=bias_ap, scalar2=7.0, op0=ALU.add, op1=ALU.min),
                                reads=[pk, "bgu"], writes=["uub%d" % j])
                            P.op("dve", lambda e, j=j: e.tensor_scalar(out=uub[j][:], in0=uub[j][:], scalar1=-7.0, scalar2=1.0,
                                                                       op0=ALU.max, op1=ALU.add), reads=["uub%d" % j], writes=["uub%d" % j])
                            ai = (sb_ - 4) * 4 + blk
                            P.op("pool", lambda e, j=j, blk=blk, ai=ai: e.tensor_tensor(out=R(actT[:, ai, :]), in0=uub[j][:], in1=gsb[:, blk, :], op=ALU.mult),
                                 reads=["uub%d" % j, "gsb%d" % blk], writes=["actT"])
                for db in range(8):
                    for kg in range(2):
                        i = wc % NWT
                        wc += 1
                        dma_kc(wt[i], w_dn[ex, kg * 1024:(kg + 1) * 1024, db * 512:(db + 1) * 512]
                               .rearrange("(kc p) c -> p kc c", p=128), 8, 4, ["wt%d" % i], "d_wt%d" % i)

                        def md(e, i=i, kg=kg, db=db):
                            for s in range(2):
                                for kc in range(8):
                                    ins = e.matmul(pst[4 + s][:], lhsT=R(actT[:, kg * 8 + kc, s * 128:(s + 1) * 128]), rhs=R(wt[i][:, kc, :]),
                                                   start=(kg == 0 and kc == 0), stop=False)
                                if kg == 1:
                                    ins = e.matmul(pst[4 + s][:], lhsT=R(ones[0:1, :]), rhs=R(bdn[0:1, db * 512:(db + 1) * 512]),
                                                   start=False, stop=True)
                            return ins
                        P.op("pe", md, reads=wk("wt%d" % i, 8, 4) + ["actT", "bdn", "ones"], writes=[PK[4], PK[5]])
                    for s in range(2):
                        ob = ost[s][db % 2]
                        ok_ = "ost%d_%d" % (s, db % 2)
                        if s == 0:
                            P.op("act", lambda e, s=s, ob=ob: e.copy(out=ob[:], in_=pst[4 + s][:]), reads=[PK[4 + s]], writes=[ok_])
                        else:
                            P.op("dve", lambda e, s=s, ob=ob: e.tensor_copy(out=ob[:], in_=pst[4 + s][:]), reads=[PK[4 + s]], writes=[ok_])
                        dma("act", Ed[ex * SLOTS + s * 128: ex * SLOTS + (s + 1) * 128, db * 512:(db + 1) * 512], ob[:], [ok_], [], "d_" + ok_)
            P.barrier()
            P.emit()

        if stop_after == 'D':
            return nc
        with ExitStack() as ph:
            xa = sbt(ph, "xa", [128, D])
            gk = [sbt(ph, "gk%d" % i, [128, D]) for i in range(2)]
            gbc = sbt(ph, "gbc", [128, D])
            bbc = sbt(ph, "bbc", [128, D])
            sqt = sbt(ph, "sqt", [128, D])
            ot = sbt(ph, "ot", [128, D])
            st1 = sbt(ph, "st1", [128, 4])
            dma("sp", gbc[:], bass.AP(ln2p_h, 0, [[0, 128], [1, D]]), [], ["gbc"], "e_g")
            dma("sp", bbc[:], bass.AP(ln2p_h, D, [[0, 128], [1, D]]), [], ["bbc"], "e_b")
            gc = 0
            outs = []
            for tt in range(8):
                dma("sp", xa[:], x1d[tt * 128:(tt + 1) * 128, :], [], ["xa"], "e_x")
                P.op("dve", lambda e: e.tensor_scalar(out=xa[:], in0=xa[:], scalar1=ALPHA, scalar2=None, op0=ALU.mult),
                     reads=["xa"], writes=["xa"])
                for k in range(4):
                    i = gc % 2
                    gc += 1
                    P.op("pool", lambda e, i=i, tt=tt, k=k: e.indirect_dma_start(
                        out=gk[i][:], out_offset=None, in_=Ed,
                        in_offset=bass.IndirectOffsetOnAxis(ap=desti[:, tt, k:k + 1], axis=0)),
                        reads=["desti"], writes=["gk%d" % i], dsem="e_gk%d" % i)
                    P.op("dve", lambda e, i=i, tt=tt, k=k: e.scalar_tensor_tensor(
                        out=xa[:], in0=gk[i][:], scalar=gates[:, tt, k:k + 1], in1=xa[:], op0=ALU.mult, op1=ALU.add),
                        reads=["gk%d" % i, "xa", "gates"], writes=["xa"])
                P.op("dve", lambda e: e.reduce_sum(out=st1[:, 0:1], in_=xa[:], axis=AX.X), reads=["xa"], writes=["st0"])
                P.op("dve", lambda e: e.tensor_scalar(out=st1[:, 1:2], in0=st1[:, 0:1], scalar1=-1.0 / D, scalar2=None, op0=ALU.mult),
                     reads=["st0"], writes=["st1"])
                P.op("dve", lambda e: e.tensor_scalar(out=xa[:], in0=xa[:], scalar1=st1[:, 1:2], scalar2=None, op0=ALU.add),
                     reads=["xa", "st1"], writes=["xa"])
                P.op("act", lambda e: e.activation(out=sqt[:], in_=xa[:], func=AF.Square), reads=["xa"], writes=["sqt"])
                P.op("dve", lambda e: e.reduce_sum(out=st1[:, 2:3], in_=sqt[:], axis=AX.X), reads=["sqt"], writes=["st2"])
                P.op("dve", lambda e: e.tensor_scalar(out=st1[:, 3:4], in0=st1[:, 2:3], scalar1=1.0 / D, scalar2=EPS, op0=ALU.mult, op1=ALU.add),
                     reads=["st2"], writes=["st3"])
                rsqrt_inplace(lambda: st1[:, 3:4], "st3")
                P.op("dve", lambda e: e.scalar_tensor_tensor(out=ot[:], in0=xa[:], scalar=st1[:, 3:4], in1=gbc[:], op0=ALU.mult, op1=ALU.mult),
                     reads=["xa", "st3", "gbc"], writes=["ot"])
                P.op("pool", lambda e: e.tensor_tensor(out=ot[:], in0=ot[:], in1=bbc[:], op=ALU.add), reads=["ot", "bbc"], writes=["ot"])
                outs.append(dma("act", out[tt * 128:(tt + 1) * 128, :], ot[:], ["ot"], [], "e_out"))
            P.barrier()
            P.emit()
    return nc


def _t5_bucket_np(d):
    n = np.maximum(d, 0)
    nf = np.maximum(n, 1).astype(np.float32)
    large = 16 + (np.log(nf / np.float32(16)) / np.float32(math.log(128 / 16)) * np.float32(16)).astype(np.int32)
    large = np.minimum(large, 31)
    return np.where(n < 16, n, large)


def _tiles(xtok):
    T = xtok.shape[0]
    return np.ascontiguousarray(xtok.reshape(T // 128, 128, 32, 128).transpose(0, 3, 2, 1))


def make_in_maps(x, mem, rel_table, w_in, w_mem_kv, w_o, lambda_q1, lambda_k1, lambda_q2, lambda_k2,
                 subln_g, conv_w, conv_b, conv_ln_g, conv_ln_b, ln1_g, ln1_b, w_router, b_router,
                 w_gate_up, b_gate_up, w_down, b_down, ln2_g, ln2_b):
    f = lambda a: np.ascontiguousarray(np.asarray(a, dtype=np.float32))
    x = f(x); mem = f(mem); rel_table = f(rel_table)
    shared = {
        "w_in": f(w_in[0]), "w_mkv": f(w_mem_kv[0]),
        "w_o": f(np.asarray(w_o[0]).reshape(32, 128, 32, 128).transpose(2, 1, 0, 3).reshape(32, 128, 4096)),
        "lamv": f(np.concatenate([lambda_q1[0], lambda_k1[0], lambda_q2[0], lambda_k2[0]])[None, :]),
        "subln": f(np.asarray(subln_g[0]).reshape(2, 128).T),
        "convw": f(np.asarray(conv_w[0])[:, 0, :].reshape(31, 8, 128).transpose(2, 1, 0)),
        "convp": f(np.stack([np.asarray(conv_b[0]).reshape(8, 128).T, np.asarray(conv_ln_g[0]).reshape(8, 128).T,
                             np.asarray(conv_ln_b[0]).reshape(8, 128).T], axis=1)),
        "ln1p": f(np.stack([np.asarray(ln1_g[0]).reshape(32, 128).T, np.asarray(ln1_b[0]).reshape(32, 128).T], axis=1)),
        "w_rt": f(w_router[0]), "b_rt": f(np.asarray(b_router[0])[None, :]),
        "w_gu": f(w_gate_up[0][:NEXP_UPLOAD]), "b_gu": f(np.asarray(b_gate_up[0]).reshape(NE, 32, 128).transpose(2, 0, 1)),
        "w_dn": f(w_down[0][:NEXP_UPLOAD]), "b_dn": f(b_down[0]),
        "ln2p": f(np.stack([np.asarray(ln2_g[0]), np.asarray(ln2_b[0])], axis=0)),
    }
    ext = np.concatenate([rel_table, np.full((1, 8), NEG, np.float32)], axis=0)
    in_maps = []
    for c in range(8):
        b, h = c // 2, c % 2
        xb = x[b]
        own = xb[h * 1024:(h + 1) * 1024]
        xr = xb.reshape(16, 128, D)[:, ::-1, :].reshape(2048, D)
        halo = xb[h * 1024 - 32: h * 1024] if h == 1 else np.zeros((32, D), np.float32)
        dd = np.arange(3072) - 2047 + h * 1024
        bidx = np.where(dd < 0, 32, _t5_bucket_np(dd))
        gvec = np.ascontiguousarray(ext[bidx].T)
        m = dict(shared)
        m.update({
            "xkv": _tiles(xr), "xq": _tiles(own),
            "xh": np.ascontiguousarray(halo.reshape(32, 32, 128).transpose(2, 1, 0)),
            "xqT": np.ascontiguousarray(own.T), "memt": _tiles(mem[b]), "gvec": gvec,
        })
        in_maps.append(m)
    return in_maps


def kernel(**inputs):
    in_maps = make_in_maps(**inputs)
    nc = build()
    res = run_bass_kernel_spmd(nc, in_maps, core_ids=list(range(8)))
    out = np.empty((4, 2048, D), np.float32)
    for c in range(8):
        b, h = c // 2, c % 2
        out[b, h * 1024:(h + 1) * 1024] = res.results[c]["out"]
    return out
```

```python
import math
from contextlib import ExitStack
import numpy as np
import concourse.bass as bass
import concourse.mybir as mybir
from concourse.bass_utils import run_bass_kernel_spmd

F32 = mybir.dt.float32
F32R = mybir.dt.float32r
I32 = mybir.dt.int32
U32 = mybir.dt.uint32
ALU = mybir.AluOpType
AF = mybir.ActivationFunctionType
AX = mybir.AxisListType

D = 4096
NE = 32
SLOTS = 256
ALPHA = 2.0 ** 0.25
LAM_INIT = 0.2
EPS = 1e-5
NEG = -30000.0
NEXP_UPLOAD = NE


def R(ap):
    return ap.bitcast(F32R)


class Prog:
    ENGS = ("pe", "act", "dve", "pool", "sp")

    def __init__(self, nc, stack):
        self.nc = nc
        self.stack = stack
        self.q = {e: [] for e in self.ENGS}
        self.psem = {}
        for e in ("pe", "act", "dve", "pool"):
            self.psem[e] = stack.enter_context(nc.semaphore("prog_" + e))
        self.cnt = {e: 0 for e in self.ENGS}
        self.waited = {e: {} for e in self.ENGS}
        self.state = {}
        self.dma_sems = {}
        self.dma_cnt = {}

    def _st(self, key):
        if key not in self.state:
            self.state[key] = {"w": None, "r": []}
        return self.state[key]

    def _prune(self, eng, deps):
        need = {}
        for sem, val in deps:
            k = id(sem)
            if self.waited[eng].get(k, 0) >= val:
                continue
            if k not in need or need[k][1] < val:
                need[k] = (sem, val)
        for k, (sem, val) in need.items():
            self.waited[eng][k] = val
        return list(need.values())

    def op(self, eng, fn, reads=(), writes=(), dsem=None):
        writes = list(writes) + [r for r in reads if r.startswith("ps") and r[2:].isdigit() and r not in writes]
        deps = []
        own = self.psem.get(eng) if dsem is None else None
        for r in reads:
            st = self._st(r)
            if st["w"] is not None:
                deps.append(st["w"])
        for w in writes:
            st = self._st(w)
            if st["w"] is not None and st["w"][0] is not own:
                deps.append(st["w"])
            for t in st["r"]:
                if t[0] is not own:
                    deps.append(t)
        deps = self._prune(eng, deps)
        if dsem is not None:
            if dsem not in self.dma_sems:
                self.dma_sems[dsem] = self.stack.enter_context(self.nc.semaphore("d_" + dsem))
                self.dma_cnt[dsem] = 0
            self.dma_cnt[dsem] += 16
            tok = (self.dma_sems[dsem], self.dma_cnt[dsem])
            inc = 16
        else:
            self.cnt[eng] += 1
            tok = (self.psem[eng], self.cnt[eng])
            inc = 1
        self.q[eng].append((deps, fn, tok[0], inc))
        for r in reads:
            self._st(r)["r"].append(tok)
        for w in writes:
            self.state[w] = {"w": tok, "r": []}
        return tok

    def barrier(self):
        toks = [(self.psem[e], self.cnt[e]) for e in ("pe", "act", "dve", "pool") if self.cnt[e] > 0]
        toks += [(self.dma_sems[n], self.dma_cnt[n]) for n in self.dma_sems]
        for e in self.ENGS:
            deps = self._prune(e, [t for t in toks if t[0] is not self.psem.get(e)])
            if deps:
                self.q[e].append((deps, None, None, 0))
        self.state = {}

    def emit(self):
        nc = self.nc
        q = self.q

        def run(e, items):
            for deps, fn, sem, inc in items:
                for s, v in deps:
                    e.wait_ge(s, v)
                if fn is not None:
                    fn(e).then_inc(sem, inc)

        with nc.Block() as block:
            @block.tensor
            def _(e):
                run(e, q["pe"])

            @block.scalar
            def _(e):
                run(e, q["act"])

            @block.vector
            def _(e):
                run(e, q["dve"])

            @block.gpsimd
            def _(e):
                run(e, q["pool"])

            @block.sync
            def _(e):
                run(e, q["sp"])
        self.q = {e: [] for e in self.ENGS}


def build(dbg=False, n_experts_run=NE, stop_after=None, n_heads=8, do_conv=True, do_mem=True, head_stage=3):
    nc = bass.Bass("TRN2", target_bir_lowering=False)
    nc.dge_precook = False

    def din(name, shape, dt=F32):
        return nc.dram_tensor(name, list(shape), dt, kind="ExternalInput")

    xkv_t = din("xkv", [16, 128, 32, 128]).ap()
    xq_t = din("xq", [8, 128, 32, 128]).ap()
    xh_t = din("xh", [128, 32, 32]).ap()
    xqT = din("xqT", [D, 1024]).ap()
    mem_t = din("memt", [2, 128, 32, 128]).ap()
    gvec_h = din("gvec", [8, 3072])
    w_in = din("w_in", [D, 9216]).ap().rearrange("(kc p) c -> p kc c", p=128)
    w_mkv = din("w_mkv", [D, 2048]).ap().rearrange("(kc p) c -> p kc c", p=128)
    w_o = din("w_o", [32, 128, 32 * 128]).ap()
    lamv_h = din("lamv", [1, 512])
    subln = din("subln", [128, 2]).ap()
    convw = din("convw", [128, 8, 31]).ap()
    convp = din("convp", [128, 3, 8]).ap()
    ln1p = din("ln1p", [128, 2, 32]).ap()
    w_rt = din("w_rt", [D, NE]).ap().rearrange("(kc p) c -> p kc c", p=128)
    b_rt = din("b_rt", [1, NE]).ap()
    w_gu = din("w_gu", [n_experts_run, 8, 4, 128, 8 * 512]).ap()
    b_gu = din("b_gu", [128, NE, 32]).ap()
    w_dn = din("w_dn", [n_experts_run, 8, 2, 128, 8 * 512]).ap()
    b_dn = din("b_dn", [NE, D]).ap()
    ln2p_h = din("ln2p", [2, D])
    out = nc.dram_tensor("out", [1024, D], F32, kind="ExternalOutput").ap()
    kd = "ExternalOutput" if dbg else "Internal"
    mixT = nc.dram_tensor("mixT", [D, 1024], F32, kind=kd).ap()
    x1d = nc.dram_tensor("x1d", [1024, D], F32, kind=kd).ap()
    Ed = nc.dram_tensor("Ed", [NE * SLOTS, D], F32, kind="Internal").ap()
    btok = nc.dram_tensor("btok", [NE * SLOTS, 1], I32, kind=kd).ap()

    with ExitStack() as top:
        P = Prog(nc, top)
        sbt = lambda st, name, shape, dt=F32: st.enter_context(nc.sbuf_tensor(name, list(shape), dt))
        pst = [top.enter_context(nc.psum_tensor("psb%d" % i, [128, 512], F32)) for i in range(8)]
        PK = ["ps%d" % i for i in range(8)]

        ident = sbt(top, "ident", [128, 128])
        ones = sbt(top, "ones", [128, 128])
        ustr = sbt(top, "ustr", [128, 128])
        neglam = sbt(top, "neglam", [128, 1])
        g8 = sbt(top, "g8", [128, 2])
        gates = sbt(top, "gates", [128, 8, 4])
        desti = sbt(top, "desti", [128, 8, 4], I32)

        cz = sbt(top, "cz", [128, 128])
        P.op("pool", lambda e: e.memset(cz[:], 0.0), writes=["cz"])
        P.op("pool", lambda e: e.affine_select(out=cz[:], in_=cz[:], pattern=[[-1, 128]],
                                               compare_op=ALU.not_equal, fill=1.0, base=0, channel_multiplier=1),
             reads=["cz"], writes=["cz"])
        P.op("dve", lambda e: e.tensor_copy(out=R(ident[:]), in_=cz[:]), reads=["cz"], writes=["ident"])
        P.op("pool", lambda e: e.memset(cz[:], 1.0), reads=[], writes=["cz"])
        P.op("dve", lambda e: e.tensor_copy(out=R(ones[:]), in_=cz[:]), reads=["cz"], writes=["ones"])
        P.op("pool", lambda e: e.affine_select(out=cz[:], in_=cz[:], pattern=[[1, 128]],
                                               compare_op=ALU.is_gt, fill=0.0, base=0, channel_multiplier=-1),
             reads=["cz"], writes=["cz"])
        P.op("dve", lambda e: e.tensor_copy(out=R(ustr[:]), in_=cz[:]), reads=["cz"], writes=["ustr"])


        def rsqrt_inplace(ap_fn, key, add_eps=False):
            if add_eps:
                P.op("dve", lambda e: e.tensor_scalar(out=ap_fn(), in0=ap_fn(), scalar1=EPS, scalar2=None, op0=ALU.add),
                     reads=[key], writes=[key])
            P.op("act", lambda e: e.activation(out=ap_fn(), in_=ap_fn(), func=AF.Sqrt), reads=[key], writes=[key])
            P.op("dve", lambda e: e.reciprocal(out=ap_fn(), in_=ap_fn()), reads=[key], writes=[key])

        def dma(eng, out_ap, in_ap, reads, writes, sem):
            return P.op(eng, lambda e, o=out_ap, i=in_ap: e.dma_start(out=o, in_=i), reads=reads, writes=writes, dsem=sem)


        pending = []

        def dma_kc(out3, in3, nk, step, writes, sem, eng="sp"):
            for k0 in range(0, nk, step):
                args = ("sp" if eng == "defer" else eng, R(out3[:, k0:k0 + step, :]), R(in3[:, k0:k0 + step, :]), [],
                        ["%s.%d" % (writes[0], k0)], sem)
                if eng == "defer":
                    pending.append(args)
                else:
                    dma(*args)

        def flush_pending(n=None):
            k = len(pending) if n is None else min(n, len(pending))
            for _ in range(k):
                dma(*pending.pop(0))

        def wk(base, nk=32, step=4):
            return ["%s.%d" % (base, k0) for k0 in range(0, nk, step)]

        with ExitStack() as ph:
            wkv = sbt(ph, "wkv", [128, 32, 512])
            wq = sbt(ph, "wq", [128, 32, 256])
            NXB = 2
            xb = [sbt(ph, "xb%d" % i, [128, 32, 128]) for i in range(NXB)]
            big = sbt(ph, "big", [128, 10240])
            KT = big[:, 0:4096].rearrange("p (m t) -> p m t", m=2)
            V = big[:, 4096:8192].rearrange("p (m t) -> p m t", m=16)
            QT = big[:, 8192:10240].rearrange("p (m t) -> p m t", m=2)
            hT = big[:, 0:8448].rearrange("p (m t) -> p m t", m=8)
            acc = wq[:].rearrange("p a b -> p (a b)").rearrange("p (m t) -> p m t", m=8)
            ksb = [sbt(ph, "ksb%d" % i, [128, 256]) for i in range(2)]
            btt = sbt(ph, "btt", [128, 2, 512])
            bt = [btt[:, i, :] for i in range(2)]
            xhb = btt[:].rearrange("p a b -> p (a b)").rearrange("p (k t) -> p k t", k=32)
            pt = [sbt(ph, "pt%d" % i, [128, 512]) for i in range(2)]
            rec = sbt(ph, "rec", [128, 512])
            o1 = sbt(ph, "o1", [128, 2, 512])
            of = sbt(ph, "of", [128, 2, 512])
            sq = sbt(ph, "sq", [128, 2, 512])
            rstd = sbt(ph, "rstd", [128, 512])
            ao = [sbt(ph, "ao%d" % i, [128, 2, 512]) for i in range(2)]
            lv = of[:, 0, :]
            lpr = of[:, 1, 0:256]
            ls = sbt(ph, "ls", [128, 4])
            sub_sb = sbt(ph, "sub_sb", [128, 2])
            cw = sbt(ph, "cw", [128, 8, 31])
            cp = sbt(ph, "cp", [128, 3, 8])
            sig = [pt[i][:, 0:256] for i in range(2)]
            mean = rec
            tmp = o1[:, 0, :]

            dma("sp", lv[:], bass.AP(lamv_h, 0, [[0, 128], [1, 512]]), [], ["lv"], "misc")
            dma("sp", sub_sb[:], subln, [], ["sub_sb"], "misc2")
            dma("sp", cw[:], convw, [], ["cw"], "misc3")
            dma("sp", cp[:], convp, [], ["cp"], "misc4")
            P.op("dve", lambda e: e.tensor_tensor(out=lpr[:, 0:128], in0=lv[:, 0:128], in1=lv[:, 128:256], op=ALU.mult),
                 reads=["lv"], writes=["lpr0"])
            P.op("dve", lambda e: e.tensor_tensor(out=lpr[:, 128:256], in0=lv[:, 256:384], in1=lv[:, 384:512], op=ALU.mult),
                 reads=["lv"], writes=["lpr1"])
            P.op("dve", lambda e: e.reduce_sum(out=ls[:, 0:1], in_=lpr[:, 0:128], axis=AX.X), reads=["lpr0"], writes=["ls0"])
            P.op("dve", lambda e: e.reduce_sum(out=ls[:, 1:2], in_=lpr[:, 128:256], axis=AX.X), reads=["lpr1"], writes=["ls1"])
            P.op("act", lambda e: e.activation(out=ls[:, 2:4], in_=ls[:, 0:2], func=AF.Exp), reads=["ls0", "ls1"], writes=["ls2"])
            P.op("dve", lambda e: e.tensor_tensor(out=neglam[:], in0=ls[:, 3:4], in1=ls[:, 2:3], op=ALU.subtract),
                 reads=["ls2"], writes=["neglam"])
            P.op("dve", lambda e: e.tensor_scalar(out=neglam[:], in0=neglam[:], scalar1=-LAM_INIT, scalar2=None, op0=ALU.add),
                 reads=["neglam"], writes=["neglam"])
            P.op("dve", lambda e: e.tensor_scalar(out=g8[:], in0=sub_sb[:], scalar1=1.0 - LAM_INIT, scalar2=None, op0=ALU.mult),
                 reads=["sub_sb"], writes=["g8"])

            xcnt = [0]

            def proj(x_tiles, n_tiles, w_tile, wkey, N, evac, halo=False):
                wkeys = (wk("wkvA") + wk("wkvB")) if wkey == "wkv" else wk(wkey)
                for t in range(n_tiles):
                    i = xcnt[0] % NXB
                    ip = xcnt[0] % 2
                    xcnt[0] += 1
                    xkey = "xb%d" % i
                    pkey = PK[ip]
                    ps = pst[ip]
                    dma("sp", R(xb[i][:].rearrange("p k t -> p (k t)")), R(x_tiles[t].rearrange("p k t -> p (k t)")), [], [xkey], xkey)

                    def mm(e, i=i, ps=ps):
                        for kc in range(32):
                            ins = e.matmul(ps[:, 0:N], lhsT=R(xb[i][:, kc, :]), rhs=R(w_tile[:, kc, 0:N]),
                                           start=(kc == 0), stop=(kc == 31))
                        return ins
                    P.op("pe", mm, reads=[xkey] + wkeys, writes=[pkey])
                    evac(t, ps, pkey, 128)
                if halo:
                    dma("sp", R(xhb[:].rearrange("p k t -> p (k t)")), R(xh_t.rearrange("p k t -> p (k t)")), [], ["xhb"], "xhb")
                    ps = pst[2]

                    def mmh(e, ps=ps):
                        for kc in range(32):
                            ins = e.matmul(ps[0:32, 0:N], lhsT=R(xhb[:, kc, :]), rhs=R(w_tile[:, kc, 0:N]),
                                           start=(kc == 0), stop=(kc == 31))
                        return ins
                    P.op("pe", mmh, reads=["xhb"] + wkeys, writes=[PK[2]])
                    evac(-1, ps, PK[2], 32)

            tcnt = [0, 0]

            def transpose_to(src_sb, src_key, dst_ap_fn, dst_key, np_=128, scale_eng="dve"):
                ps = pst[3]

                def tr(e):
                    for m in range(2):
                        ins = e.transpose(ps[:, m * 128:m * 128 + np_], src_sb[0:np_, m * 128:(m + 1) * 128],
                                          ident[0:np_, 0:np_])
                    return ins
                P.op("pe", tr, reads=[src_key, "ident"], writes=[PK[3]])
                src = ps[:, 0:256].rearrange("p (m t) -> p m t", m=2)[:, :, 0:np_]
                P.op(scale_eng, lambda e: e.tensor_copy(out=R(dst_ap_fn()), in_=src), reads=[PK[3]], writes=[dst_key])

            def attention(hd_rows, n_dc, nkt_fn, bias_hd, nmaps, post):
                accb = [(6, 7, 2), (0, 1, 3)]
                for c in range(2):
                    nk = nkt_fn(c)

                    def emit_lg(j, c=c):
                        slots = []
                        bj = None
                        if bias_hd is not None:
                            bj = tcnt[1] % 2
                            tcnt[1] += 1
                            off = 1920 + 512 * c - 128 * j
                            dma("sp", R(bt[bj][:]), R(bass.AP(gvec_h, bias_hd * 3072 + off, [[1, 128], [1, 512]])),
                                [], ["bt%d" % bj], "bt%d" % bj)
                            flush_pending(1)
                        for m in range(nmaps):
                            jj = tcnt[0] % 2
                            tcnt[0] += 1
                            psl = pst[4 + jj]
                            plk = PK[4 + jj]
                            if bias_hd is not None:
                                def lg(e, psl=psl, m=m, j=j, bj=bj):
                                    e.matmul(psl[:], lhsT=R(KT[:, m, j * 128:(j + 1) * 128]),
                                             rhs=R(QT[:, m, c * 512:(c + 1) * 512]), start=True, stop=False)
                                    return e.matmul(psl[:], lhsT=R(ident[:]), rhs=R(bt[bj][:]), start=False, stop=True)
                                P.op("pe", lg, reads=["KT", "QT", "bt%d" % bj, "ident"], writes=[plk])
                            else:
                                def lg(e, psl=psl, j=j):
                                    for dc in range(n_dc):
                                        ins = e.matmul(psl[:], lhsT=R(KT[:, dc, j * 128:(j + 1) * 128]),
                                                       rhs=R(QT[:, dc, c * 512:(c + 1) * 512]),
                                                       start=(dc == 0), stop=(dc == n_dc - 1))
                                    return ins
                                P.op("pe", lg, reads=["KT", "QT"], writes=[plk])
                            P.op("act", lambda e, jj=jj, psl=psl: e.activation(out=R(pt[jj][:]), in_=psl[:], func=AF.Exp),
                                 reads=[plk], writes=["pt%d" % jj])
                            slots.append(jj)
                        return slots

                    def emit_pv(j, slots, nk=nk):
                        for m in range(nmaps):
                            jj = slots[m]
                            b0, b1, b2 = accb[m]

                            def pv(e, j=j, jj=jj, b0=b0, b1=b1, b2=b2):
                                e.matmul(pst[b0][:], lhsT=R(V[:, j, 0:128]), rhs=R(pt[jj][:]), start=(j == 0), stop=(j == nk - 1))
                                e.matmul(pst[b1][:], lhsT=R(V[:, j, 128:256]), rhs=R(pt[jj][:]), start=(j == 0), stop=(j == nk - 1))
                                return e.matmul(pst[b2][:], lhsT=R(ones[:]), rhs=R(pt[jj][:]), start=(j == 0), stop=(j == nk - 1))
                            P.op("pe", pv, reads=["V", "pt%d" % jj, "ones"], writes=[PK[b0], PK[b1], PK[b2]])

                    if nmaps == 2:
                        prev = emit_lg(0)
                        for j in range(nk):
                            emit_pv(j, prev)
                            prev = emit_lg(j + 1) if j + 1 < nk else None
                    else:
                        prev = emit_lg(0)
                        for j in range(nk):
                            nxt = emit_lg(j + 1) if j + 1 < nk else None
                            emit_pv(j, prev)
                            prev = nxt
                    for m in range(nmaps):
                        b0, b1, b2 = accb[m]
                        P.op("dve", lambda e, b2=b2: e.reciprocal(out=rec[:], in_=pst[b2][:]), reads=[PK[b2]], writes=["rec"])
                        dst = o1 if (nmaps == 2 and m == 0) else of
                        dk = "o1" if (nmaps == 2 and m == 0) else "of"
                        for dvc, bb in enumerate((b0, b1)):
                            P.op("dve", lambda e, dvc=dvc, dst=dst, bb=bb: e.tensor_tensor(
                                out=dst[:, dvc, :], in0=pst[bb][:], in1=rec[:], op=ALU.mult),
                                reads=[PK[bb], "rec"], writes=[dk + str(dvc)])
                    post(c)
                flush_pending()

            ocnt = [0]

            def store_mix(row0, c, src_tile, src_keys):
                for dvc in range(2):
                    dma("act", mixT[row0 + dvc * 128: row0 + (dvc + 1) * 128, c * 512:(c + 1) * 512],
                        src_tile[:, dvc, :], [src_keys[dvc]], [], "st_" + src_keys[dvc])

            def load_head_w(hd, eng="sp"):
                dma_kc(wkv[:, :, 0:256], w_in[:, :, 2048 + hd * 256: 2048 + (hd + 1) * 256], 32, 4, ["wkvA"], "wkv", eng)
                dma_kc(wkv[:, :, 256:512], w_in[:, :, 4096 + hd * 256: 4096 + (hd + 1) * 256], 32, 4, ["wkvB"], "wkv", eng)
                dma_kc(wq, w_in[:, :, hd * 256:(hd + 1) * 256], 32, 4, ["wq"], "wq", eng)

            def load_conv_w(ci, eng="sp"):
                dma_kc(wkv[:, :, 0:256], w_in[:, :, 6144 + ci * 256: 6144 + (ci + 1) * 256], 32, 4, ["wkvA"], "wkv", eng)
                dma_kc(wkv[:, :, 256:512], w_in[:, :, 7168 + ci * 256: 7168 + (ci + 1) * 256], 32, 4, ["wkvB"], "wkv", eng)

            def load_mem_w(hd, which, eng="sp"):
                if which in ("kv", "all"):
                    dma_kc(wkv[:, :, 0:256], w_mkv[:, :, hd * 256:(hd + 1) * 256], 32, 4, ["wkvA"], "wkv", eng)
                    dma_kc(wkv[:, :, 256:512], w_mkv[:, :, 1024 + hd * 256: 1024 + (hd + 1) * 256], 32, 4, ["wkvB"], "wkv", eng)
                if which in ("q", "all"):
                    dma_kc(wq, w_in[:, :, 8192 + hd * 256: 8192 + (hd + 1) * 256], 32, 4, ["wq"], "wq", eng)

            for hd in range(n_heads):
                if hd == 0:
                    load_head_w(0)

                def evac_kv(t, ps, pkey, np_):
                    i = t % 2
                    P.op("act", lambda e, i=i, ps=ps: e.copy(out=ksb[i][:], in_=ps[:, 0:256]), reads=[pkey], writes=["ksb%d" % i])
                    P.op("dve", lambda e, t=t, ps=ps: e.tensor_copy(out=R(V[:, t, :]), in_=ps[:, 256:512]), reads=[pkey], writes=["V"])
                    transpose_to(ksb[i], "ksb%d" % i, lambda t=t: KT[:, :, t * 128:(t + 1) * 128], "KT")
                proj(xkv_t, 16, wkv, "wkv", 512, evac_kv)

                def evac_q(t, ps, pkey, np_):
                    i = t % 2
                    P.op("act", lambda e, i=i, ps=ps: e.mul(out=ksb[i][:], in_=ps[:, 0:256], mul=128.0 ** -0.5),
                         reads=[pkey], writes=["ksb%d" % i])
                    transpose_to(ksb[i], "ksb%d" % i, lambda t=t: QT[:, :, t * 128:(t + 1) * 128], "QT")
                if head_stage >= 2:
                    proj(xq_t, 8, wq, "wq", 256, evac_q)

                def post_diff(c, hd=hd):
                    for dvc in range(2):
                        P.op("dve", lambda e, dvc=dvc: e.scalar_tensor_tensor(
                            out=of[:, dvc, :], in0=of[:, dvc, :], scalar=neglam[:, 0:1], in1=o1[:, dvc, :],
                            op0=ALU.mult, op1=ALU.add), reads=["of%d" % dvc, "o1%d" % dvc, "neglam"], writes=["of%d" % dvc])
                        P.op("act", lambda e, dvc=dvc: e.activation(out=R(sq[:, dvc, :]), in_=of[:, dvc, :], func=AF.Square),
                             reads=["of%d" % dvc], writes=["sq%d" % dvc])

                    def ssum(e):
                        e.matmul(pst[3][:], lhsT=R(ones[:]), rhs=R(sq[:, 0, :]), start=True, stop=False)
                        return e.matmul(pst[3][:], lhsT=R(ones[:]), rhs=R(sq[:, 1, :]), start=False, stop=True)
                    P.op("pe", ssum, reads=["sq0", "sq1", "ones"], writes=[PK[3]])
                    P.op("dve", lambda e: e.tensor_scalar(out=rstd[:], in0=pst[3][:], scalar1=1.0 / 256, scalar2=EPS,
                                                          op0=ALU.mult, op1=ALU.add), reads=[PK[3]], writes=["rstd"])
                    rsqrt_inplace(lambda: rstd[:], "rstd")
                    a = ao[ocnt[0] % 2]
                    ak = "ao%d_" % (ocnt[0] % 2)
                    ocnt[0] += 1
                    for dvc in range(2):
                        P.op("dve", lambda e, dvc=dvc, a=a: e.scalar_tensor_tensor(
                            out=a[:, dvc, :], in0=of[:, dvc, :], scalar=g8[:, dvc:dvc + 1], in1=rstd[:],
                            op0=ALU.mult, op1=ALU.mult), reads=["of%d" % dvc, "g8", "rstd"], writes=[ak + str(dvc)])
                    store_mix(hd * 256, c, a, [ak + "0", ak + "1"])

                if hd + 1 < n_heads:
                    load_head_w(hd + 1, "defer")
                elif do_conv:
                    load_conv_w(0, "defer")
                if head_stage >= 3:
                    attention(hd * 256, 1, lambda c: 12 + 4 * c, hd, 2, post_diff)

            P.barrier()
            for ci in range(4 if do_conv else 0):
                if ci > 0 or n_heads == 0:
                    load_conv_w(ci)

                def evac_glu(t, ps, pkey, np_, ci=ci):
                    i = (t + 2) % 2
                    P.op("act", lambda e, i=i, ps=ps: e.activation(out=R(sig[i][0:np_, :]), in_=ps[0:np_, 256:512], func=AF.Sigmoid),
                         reads=[pkey], writes=["pt%d" % i])
                    P.op("dve", lambda e, i=i, ps=ps: e.tensor_tensor(out=ksb[i][0:np_, :], in0=ps[0:np_, 0:256], in1=sig[i][0:np_, :],
                                                                     op=ALU.mult), reads=[pkey, "pt%d" % i], writes=["ksb%d" % i])
                    if t >= 0:
                        dst = lambda t=t: hT[:, 2 * ci:2 * ci + 2, 32 + t * 128: 32 + (t + 1) * 128]
                    else:
                        dst = lambda: hT[:, 2 * ci:2 * ci + 2, 0:32]
                    transpose_to(ksb[i], "ksb%d" % i, dst, "hT%d" % ci, np_=np_)
                proj(xq_t, 8, wkv, "wkv", 512, evac_glu, halo=True)

            if do_conv and do_mem:
                load_mem_w(0, "kv")
            for ch in range(8 if do_conv else 0):
                eng = "dve"
                hk = "hT%d" % (ch // 2)
                akey = "acc%d" % ch
                P.op(eng, lambda e, ch=ch: e.tensor_scalar(out=R(acc[:, ch, :]), in0=hT[:, ch, 2:1026], scalar1=cw[:, ch, 0:1],
                                                           scalar2=None, op0=ALU.mult), reads=[hk, "cw"], writes=[akey])
                for j in range(1, 31):
                    P.op(eng, lambda e, ch=ch, j=j: e.scalar_tensor_tensor(
                        out=R(acc[:, ch, :]), in0=hT[:, ch, 2 + j:1026 + j], scalar=cw[:, ch, j:j + 1], in1=acc[:, ch, :],
                        op0=ALU.mult, op1=ALU.add), reads=[hk, "cw", akey], writes=[akey])
                P.op(eng, lambda e, ch=ch: e.tensor_scalar(out=R(acc[:, ch, :]), in0=acc[:, ch, :], scalar1=cp[:, 0, ch:ch + 1],
                                                           scalar2=None, op0=ALU.add), reads=[akey, "cp"], writes=[akey])
            for c in range(2 if do_conv else 0):
                cs = slice(c * 512, (c + 1) * 512)
                for ch in range(8):
                    P.op("act", lambda e, ch=ch, cs=cs: e.activation(out=R(sq[:, ch % 2, :]), in_=acc[:, ch, cs], func=AF.Square),
                         reads=["acc%d" % ch], writes=["sq%d" % (ch % 2)])
                    P.op("pe", lambda e, ch=ch, cs=cs: e.matmul(pst[2][:], lhsT=R(ones[:]), rhs=R(acc[:, ch, cs]),
                                                                start=(ch == 0), stop=(ch == 7)),
                         reads=["acc%d" % ch, "ones"], writes=[PK[2]])
                    P.op("pe", lambda e, ch=ch: e.matmul(pst[3][:], lhsT=R(ones[:]), rhs=R(sq[:, ch % 2, :]),
                                                         start=(ch == 0), stop=(ch == 7)),
                         reads=["sq%d" % (ch % 2), "ones"], writes=[PK[3]])
                P.op("dve", lambda e: e.tensor_scalar(out=mean[:], in0=pst[2][:], scalar1=1.0 / 1024, scalar2=None, op0=ALU.mult),
                     reads=[PK[2]], writes=["mean"])
                P.op("dve", lambda e: e.tensor_tensor(out=tmp[:], in0=mean[:], in1=mean[:], op=ALU.mult), reads=["mean"], writes=["tmp"])
                P.op("dve", lambda e: e.scalar_tensor_tensor(out=rstd[:], in0=pst[3][:], scalar=1.0 / 1024, in1=tmp[:],
                                                             op0=ALU.mult, op1=ALU.subtract), reads=[PK[3], "tmp"], writes=["rstd"])
                rsqrt_inplace(lambda rstd=rstd: rstd[:], "rstd", add_eps=True)
                for ch in range(8):
                    a = ao[ocnt[0] % 2]
                    ak = "ao%d_" % (ocnt[0] % 2)
                    if ch % 2 == 1:
                        ocnt[0] += 1
                    d2 = ch % 2
                    P.op("dve", lambda e, ch=ch, cs=cs: e.tensor_tensor(out=tmp[:], in0=acc[:, ch, cs], in1=mean[:], op=ALU.subtract),
                         reads=["acc%d" % ch, "mean"], writes=["tmp"])
                    P.op("dve", lambda e: e.tensor_tensor(out=tmp[:], in0=tmp[:], in1=rstd[:], op=ALU.mult),
                         reads=["tmp", "rstd"], writes=["tmp"])
                    P.op("act", lambda e, ch=ch, a=a, d2=d2: e.activation(out=a[:, d2, :], in_=tmp[:], func=AF.Silu,
                                                                         bias=cp[:, 2, ch:ch + 1], scale=cp[:, 1, ch:ch + 1]),
                         reads=["tmp", "cp"], writes=[ak + str(d2)])
                    dma("act", mixT[2048 + ch * 128: 2048 + (ch + 1) * 128, cs], a[:, d2, :], [ak + str(d2)], [], "st_" + ak + str(d2))

            P.barrier()
            for hd in range(4 if do_mem else 0):
                if hd == 0:
                    load_mem_w(0, "q" if do_conv else "all")

                def evac_mkv(t, ps, pkey, np_):
                    i = t % 2
                    P.op("act", lambda e, i=i, ps=ps: e.copy(out=ksb[i][:], in_=ps[:, 0:256]), reads=[pkey], writes=["ksb%d" % i])
                    P.op("dve", lambda e, t=t, ps=ps: e.tensor_copy(out=R(V[:, t, :]), in_=ps[:, 256:512]), reads=[pkey], writes=["V"])
                    transpose_to(ksb[i], "ksb%d" % i, lambda t=t: KT[:, :, t * 128:(t + 1) * 128], "KT")
                proj(mem_t, 2, wkv, "wkv", 512, evac_mkv)

                def evac_mq(t, ps, pkey, np_):
                    i = t % 2
                    P.op("act", lambda e, i=i, ps=ps: e.mul(out=ksb[i][:], in_=ps[:, 0:256], mul=256.0 ** -0.5),
                         reads=[pkey], writes=["ksb%d" % i])
                    transpose_to(ksb[i], "ksb%d" % i, lambda t=t: QT[:, :, t * 128:(t + 1) * 128], "QT")
                proj(xq_t, 8, wq, "wq", 256, evac_mq)

                def post_mem(c, hd=hd):
                    a = ao[ocnt[0] % 2]
                    ak = "ao%d_" % (ocnt[0] % 2)
                    ocnt[0] += 1
                    for dvc in range(2):
                        P.op("act", lambda e, dvc=dvc, a=a: e.copy(out=a[:, dvc, :], in_=of[:, dvc, :]),
                             reads=["of%d" % dvc], writes=[ak + str(dvc)])
                    store_mix(3072 + hd * 256, c, a, [ak + "0", ak + "1"])
                if hd + 1 < 4:
                    load_mem_w(hd + 1, "all")
                attention(0, 2, lambda c: 2, None, 1, post_mem)

            P.barrier()
            P.emit()

        if stop_after == 'A':
            return nc
        with ExitStack() as ph:
            mix = sbt(ph, "mix", [128, 32, 512])
            yT = sbt(ph, "yT", [128, 32, 512])
            wo = [sbt(ph, "wo%d" % i, [128, 32, 128]) for i in range(2)]
            xt = [sbt(ph, "xt%d" % i, [128, 512]) for i in range(2)]
            sqb = [sbt(ph, "sqb%d" % i, [128, 512]) for i in range(2)]
            mean = sbt(ph, "mean1", [128, 512])
            rstd = sbt(ph, "rstd1", [128, 512])
            tmp = [sbt(ph, "tmp1_%d" % i, [128, 512]) for i in range(2)]
            l1 = sbt(ph, "l1", [128, 2, 32])
            wr = sbt(ph, "wr", [128, 32, NE])
            brt = sbt(ph, "brt", [1, NE])
            x1s = [sbt(ph, "x1s0", [128, D])]
            lg = sbt(ph, "lg", [128, NE])
            mx8 = sbt(ph, "mx8", [128, 8, 8])
            mi8 = sbt(ph, "mi8", [128, 8, 8], U32)
            idxf = sbt(ph, "idxf", [128, 8, 4])
            negm = sbt(ph, "negm", [128, 8])
            esum = sbt(ph, "esum", [128, 8])
            maskall = sbt(ph, "maskall", [128, 8, NE])
            posall = sbt(ph, "posall", [128, 8, NE])
            iot = sbt(ph, "iot", [128, NE])
            oh = sbt(ph, "oh", [128, NE])
            possel = sbt(ph, "possel", [128, 8, 4])
            destf = sbt(ph, "destf", [128, 8, 4])
            tokid = sbt(ph, "tokid", [128, 8], I32)
            zt = sbt(ph, "zt", [128, 64], I32)

            dma("sp", l1[:], ln1p, [], ["l1"], "c_l1")
            dma_kc(wr, w_rt, 32, 4, ["wr"], "c_wr")
            dma("sp", R(brt[:]), R(b_rt), [], ["brt"], "c_brt")
            P.op("pool", lambda e: e.iota(iot[:], pattern=[[1, NE]], base=0, channel_multiplier=0,
                                           allow_small_or_imprecise_dtypes=True), writes=["iot"])
            P.op("pool", lambda e: e.iota(tokid[:], pattern=[[128, 8]], base=0, channel_multiplier=1), writes=["tokid"])
            P.op("pool", lambda e: e.memset(zt[:], 0), writes=["zt"])
            dma("sp", btok.rearrange("(p f) o -> p (f o)", p=128), zt[:], ["zt"], ["btok"], "c_bt")

            wcnt = 0
            x1cnt = 0
            for tg in range(2):
                ts_ = slice(tg * 512, (tg + 1) * 512)
                for q4 in range(8):
                    dma("sp", R(mix[:, q4 * 4:(q4 + 1) * 4, :]),
                        R(mixT[q4 * 512:(q4 + 1) * 512, ts_].rearrange("(kc p) t -> p kc t", p=128)),
                        [], ["mix.%d" % q4], "c_mix")
                for cb in range(32):
                    i = wcnt % 2
                    wcnt += 1
                    dma("sp", R(wo[i][:].rearrange("p k c -> p (k c)")), R(w_o[cb]), [], ["wo%d" % i], "c_wo%d" % i)
                    dma("sp", xt[i][:], xqT[cb * 128:(cb + 1) * 128, ts_], [], ["xt%d" % i], "c_xt%d" % i)
                    ps = pst[i]

                    def mmo(e, i=i, ps=ps):
                        for kc in range(32):
                            ins = e.matmul(ps[:], lhsT=R(wo[i][:, kc, :]), rhs=R(mix[:, kc, :]), start=(kc == 0), stop=(kc == 31))
                        return ins
                    P.op("pe", mmo, reads=["wo%d" % i] + ["mix.%d" % q for q in range(8)], writes=[PK[i]])
                    P.op("dve", lambda e, i=i, ps=ps, cb=cb: e.scalar_tensor_tensor(
                        out=R(yT[:, cb, :]), in0=xt[i][:], scalar=ALPHA, in1=ps[:], op0=ALU.mult, op1=ALU.add),
                        reads=["xt%d" % i, PK[i]], writes=["yT%d" % cb])
                    P.op("act", lambda e, i=i, cb=cb: e.activation(out=R(sqb[i][:]), in_=yT[:, cb, :], func=AF.Square),
                         reads=["yT%d" % cb], writes=["sqb%d" % i])
                    P.op("pe", lambda e, cb=cb: e.matmul(pst[2][:], lhsT=R(ones[:]), rhs=R(yT[:, cb, :]), start=(cb == 0), stop=(cb == 31)),
                         reads=["yT%d" % cb, "ones"], writes=[PK[2]])
                    P.op("pe", lambda e, cb=cb, i=i: e.matmul(pst[3][:], lhsT=R(ones[:]), rhs=R(sqb[i][:]), start=(cb == 0), stop=(cb == 31)),
                         reads=["sqb%d" % i, "ones"], writes=[PK[3]])
                P.op("dve", lambda e: e.tensor_scalar(out=mean[:], in0=pst[2][:], scalar1=1.0 / D, scalar2=None, op0=ALU.mult),
                     reads=[PK[2]], writes=["mean"])
                P.op("dve", lambda e: e.tensor_tensor(out=tmp[0][:], in0=mean[:], in1=mean[:], op=ALU.mult), reads=["mean"], writes=["tmp0"])
                P.op("dve", lambda e: e.scalar_tensor_tensor(out=rstd[:], in0=pst[3][:], scalar=1.0 / D, in1=tmp[0][:],
                                                             op0=ALU.mult, op1=ALU.subtract), reads=[PK[3], "tmp0"], writes=["rstd"])
                rsqrt_inplace(lambda rstd=rstd: rstd[:], "rstd", add_eps=True)
                for cb in range(32):
                    i = cb % 2
                    eng = "dve" if cb % 2 == 0 else "pool"
                    P.op(eng, lambda e, cb=cb, i=i: e.tensor_tensor(out=tmp[i][:], in0=yT[:, cb, :], in1=mean[:], op=ALU.subtract),
                         reads=["yT%d" % cb, "mean"], writes=["tmp%d" % i])
                    P.op(eng, lambda e, i=i: e.tensor_tensor(out=tmp[i][:], in0=tmp[i][:], in1=rstd[:], op=ALU.mult),
                         reads=["tmp%d" % i, "rstd"], writes=["tmp%d" % i])
                    P.op("act", lambda e, cb=cb, i=i: e.activation(out=R(yT[:, cb, :]), in_=tmp[i][:], func=AF.Identity,
                                                                  bias=l1[:, 1, cb:cb + 1], scale=l1[:, 0, cb:cb + 1]),
                         reads=["tmp%d" % i, "l1"], writes=["yT%d" % cb])
                for t4 in range(4):
                    tt = tg * 4 + t4
                    tsl = slice(t4 * 128, (t4 + 1) * 128)

                    def rt(e, tsl=tsl):
                        for cb in range(32):
                            e.matmul(pst[4][:, 0:NE], lhsT=R(yT[:, cb, tsl]), rhs=R(wr[:, cb, :]), start=(cb == 0), stop=False)
                        return e.matmul(pst[4][:, 0:NE], lhsT=R(ones[0:1, :]), rhs=R(brt[:]), start=False, stop=True)
                    P.op("pe", rt, reads=["yT%d" % cb for cb in range(32)] + wk("wr") + ["brt", "ones"], writes=[PK[4]])
                    P.op("dve", lambda e: e.tensor_copy(out=lg[:], in_=pst[4][:, 0:NE]), reads=[PK[4]], writes=["lg"])
                    P.op("dve", lambda e, tt=tt: e.max(out=mx8[:, tt, :], in_=lg[:]), reads=["lg"], writes=["mx8"])
                    P.op("dve", lambda e, tt=tt: e.max_index(out=mi8[:, tt, :], in_max=mx8[:, tt, :], in_values=lg[:]),
                         reads=["lg", "mx8"], writes=["mi8"])
                    P.op("dve", lambda e, tt=tt: e.tensor_scalar(out=R(maskall[:, tt, :]), in0=lg[:], scalar1=mx8[:, tt, 3:4],
                                                                 scalar2=None, op0=ALU.is_ge), reads=["lg", "mx8"], writes=["maskall"])
                    P.op("dve", lambda e, tt=tt: e.tensor_scalar(out=negm[:, tt:tt + 1], in0=mx8[:, tt, 0:1], scalar1=-1.0,
                                                                 scalar2=None, op0=ALU.mult), reads=["mx8"], writes=["negm"])
                    P.op("act", lambda e, tt=tt: e.activation(out=gates[:, tt, :], in_=mx8[:, tt, 0:4], func=AF.Exp,
                                                              bias=negm[:, tt:tt + 1], scale=1.0), reads=["mx8", "negm"], writes=["gates"])
                    P.op("dve", lambda e, tt=tt: e.reduce_sum(out=esum[:, tt:tt + 1], in_=gates[:, tt, :], axis=AX.X),
                         reads=["gates"], writes=["esum"])
                    P.op("dve", lambda e, tt=tt: e.reciprocal(out=esum[:, tt:tt + 1], in_=esum[:, tt:tt + 1]),
                         reads=["esum"], writes=["esum"])
                    P.op("dve", lambda e, tt=tt: e.tensor_scalar(out=gates[:, tt, :], in0=gates[:, tt, :], scalar1=esum[:, tt:tt + 1],
                                                                 scalar2=None, op0=ALU.mult), reads=["gates", "esum"], writes=["gates"])
                    P.op("dve", lambda e, tt=tt: e.tensor_copy(out=idxf[:, tt, :], in_=mi8[:, tt, 0:4]), reads=["mi8"], writes=["idxf"])
                    xs = x1s[0]
                    xk = "x1s0"
                    x1cnt += 1
                    for g4 in range(8):
                        pb = pst[5 + g4 % 2]
                        pbk = PK[5 + g4 % 2]

                        def trx(e, g4=g4, pb=pb, tsl=tsl):
                            for u in range(4):
                                ins = e.transpose(pb[:, u * 128:(u + 1) * 128], yT[:, g4 * 4 + u, tsl], ident[:])
                            return ins
                        P.op("pe", trx, reads=["yT%d" % (g4 * 4 + u) for u in range(4)] + ["ident"], writes=[pbk])
                        P.op("act", lambda e, g4=g4, pb=pb, xs=xs: e.copy(out=xs[:, g4 * 512:(g4 + 1) * 512], in_=pb[:]),
                             reads=[pbk], writes=[xk])
                    dma("act", x1d[tt * 128:(tt + 1) * 128, :], xs[:], [xk], ["x1d"], "c_" + xk)

            for tt in range(8):
                def pm(e, tt=tt):
                    for t2 in range(tt):
                        e.matmul(pst[4][:, 0:NE], lhsT=R(ones[:]), rhs=R(maskall[:, t2, :]), start=(t2 == 0), stop=False)
                    return e.matmul(pst[4][:, 0:NE], lhsT=R(ustr[:]), rhs=R(maskall[:, tt, :]), start=(tt == 0), stop=True)
                P.op("pe", pm, reads=["maskall", "ones", "ustr"], writes=[PK[4]])
                P.op("dve", lambda e, tt=tt: e.tensor_copy(out=posall[:, tt, :], in_=pst[4][:, 0:NE]), reads=[PK[4]], writes=["posall"])
                for k in range(4):
                    P.op("dve", lambda e, tt=tt, k=k: e.tensor_scalar(out=oh[:], in0=iot[:], scalar1=idxf[:, tt, k:k + 1], scalar2=None,
                                                                      op0=ALU.is_equal), reads=["iot", "idxf"], writes=["oh"])
                    P.op("dve", lambda e, tt=tt: e.tensor_tensor(out=oh[:], in0=oh[:], in1=posall[:, tt, :], op=ALU.mult),
                         reads=["oh", "posall"], writes=["oh"])
                    P.op("dve", lambda e, tt=tt, k=k: e.reduce_sum(out=possel[:, tt, k:k + 1], in_=oh[:], axis=AX.X),
                         reads=["oh"], writes=["possel"])
            P.op("dve", lambda e: e.scalar_tensor_tensor(out=destf[:], in0=idxf[:], scalar=float(SLOTS), in1=possel[:],
                                                         op0=ALU.mult, op1=ALU.add), reads=["idxf", "possel"], writes=["destf"])
            P.op("dve", lambda e: e.tensor_copy(out=desti[:], in_=destf[:]), reads=["destf"], writes=["desti"])
            for tt in range(8):
                for k in range(4):
                    P.op("pool", lambda e, tt=tt, k=k: e.indirect_dma_start(
                        out=btok, out_offset=bass.IndirectOffsetOnAxis(ap=desti[:, tt, k:k + 1], axis=0),
                        in_=tokid[:, tt:tt + 1], in_offset=None), reads=["desti", "tokid", "btok"], writes=["btok_s"], dsem="c_sc")
            P.barrier()
            P.emit()

        if stop_after == 'C':
            return nc
        with ExitStack() as ph:
            idxe = sbt(ph, "idxe", [128, 2], I32)
            xe = [sbt(ph, "xe%d" % i, [128, D]) for i in range(2)]
            xeT = sbt(ph, "xeT", [128, 32, SLOTS])
            NWT = 5
            wt = [sbt(ph, "wt%d" % i, [128, 8, 512]) for i in range(NWT)]
            actT = sbt(ph, "actT", [128, 16, SLOTS])
            gsb = sbt(ph, "gsb", [128, 4, SLOTS])
            sgb = [sbt(ph, "sgb%d" % i, [128, SLOTS]) for i in range(2)]
            uub = [sbt(ph, "uub%d" % i, [128, SLOTS]) for i in range(2)]
            ost = [[sbt(ph, "ost%d_%d" % (i, j), [128, 512]) for j in range(2)] for i in range(2)]
            bdn = sbt(ph, "bdn", [1, D])
            bgu = sbt(ph, "bgu", [128, NE, 32])
            dma("sp", bgu[:], b_gu, [], ["bgu"], "d_bgu")
            wc = 0
            for ex in range(n_experts_run):
                for s in range(2):
                    dma("sp", idxe[:, s:s + 1], btok[ex * SLOTS + s * 128: ex * SLOTS + (s + 1) * 128, :], [], ["idxe%d" % s], "d_idx%d" % s)
                    P.op("pool", lambda e, s=s: e.indirect_dma_start(
                        out=xe[s][:], out_offset=None, in_=x1d,
                        in_offset=bass.IndirectOffsetOnAxis(ap=idxe[:, s:s + 1], axis=0)),
                        reads=["idxe%d" % s], writes=["xe%d" % s], dsem="d_g%d" % s)
                dma("sp", R(bdn[:]), R(b_dn[ex:ex + 1, :]), [], ["bdn"], "d_bdn")
                for s in range(2):
                    for g4 in range(8):
                        pb = pst[6 + g4 % 2]
                        pbk = PK[6 + g4 % 2]

                        def trx(e, g4=g4, pb=pb, s=s):
                            for u in range(4):
                                ins = e.transpose(pb[:, u * 128:(u + 1) * 128], xe[s][:, (g4 * 4 + u) * 128:(g4 * 4 + u + 1) * 128], ident[:])
                            return ins
                        P.op("pe", trx, reads=["xe%d" % s, "ident"], writes=[pbk])
                        P.op("act", lambda e, g4=g4, pb=pb, s=s: e.copy(
                            out=R(xeT[:, g4 * 4:(g4 + 1) * 4, s * 128:(s + 1) * 128]),
                            in_=pb[:].rearrange("p (u t) -> p u t", u=4)), reads=[pbk], writes=["xeT"])
                for sbi, sb_ in enumerate([0, 4, 1, 5, 2, 6, 3, 7]):
                    is_up = sb_ >= 4
                    pg = (sbi % 2) * 2
                    for kg in range(4):
                        i = wc % NWT
                        wc += 1
                        dma("sp", R(wt[i][:].rearrange("p k c -> p (k c)")), R(w_gu[ex, sb_, kg]), [], ["wt%d" % i], "d_wt%d" % i)

                        def mg(e, i=i, kg=kg, pg=pg):
                            for blk in range(4):
                                po = pst[blk][:, 0:256]
                                for kc in range(8):
                                    ins = e.matmul(po, lhsT=R(wt[i][:, kc, blk * 128:(blk + 1) * 128]), rhs=R(xeT[:, kg * 8 + kc, :]),
                                                   start=(kg == 0 and kc == 0), stop=(kg == 3 and kc == 7))
                            return ins
                        P.op("pe", mg, reads=["wt%d" % i, "xeT"], writes=[PK[0], PK[1], PK[2], PK[3]])
                    for blk in range(4):
                        po = pst[blk][:, 0:256]
                        pk = PK[blk]
                        cbi = sb_ * 4 + blk
                        bias_ap = bgu[:, ex, cbi:cbi + 1]
                        if not is_up:
                            P.op("dve", lambda e, po=po, blk=blk, bias_ap=bias_ap: e.tensor_scalar(
                                out=gsb[:, blk, :], in0=po, scalar1=bias_ap, scalar2=7.0, op0=ALU.add, op1=ALU.min),
                                reads=[pk, "bgu"], writes=["gsb%d" % blk])
                            j = blk % 2
                            P.op("act", lambda e, blk=blk, j=j: e.activation(out=sgb[j][:], in_=gsb[:, blk, :], func=AF.Sigmoid, scale=1.702),
                                 reads=["gsb%d" % blk], writes=["sgb%d" % j])
                            P.op("pool", lambda e, blk=blk, j=j: e.tensor_tensor(out=gsb[:, blk, :], in0=gsb[:, blk, :], in1=sgb[j][:], op=ALU.mult),
                                 reads=["gsb%d" % blk, "sgb%d" % j], writes=["gsb%d" % blk])
                        else:
                            j = blk % 2
                            P.op("dve", lambda e, po=po, j=j, bias_ap=bias_ap: e.tensor_scalar(
                                out=uub[j][:], in0=po, scalar1=bias_ap, scalar2=7.0, op0=ALU.add, op1=ALU.min),
                                reads=[pk, "bgu"], writes=["uub%d" % j])
                            P.op("dve", lambda e, j=j: e.tensor_scalar(out=uub[j][:], in0=uub[j][:], scalar1=-7.0, scalar2=1.0,
                                                                       op0=ALU.max, op1=ALU.add), reads=["uub%d" % j], writes=["uub%d" % j])
                            ai = (sb_ - 4) * 4 + blk
                            P.op("pool", lambda e, j=j, blk=blk, ai=ai: e.tensor_tensor(out=R(actT[:, ai, :]), in0=uub[j][:], in1=gsb[:, blk, :], op=ALU.mult),
                                 reads=["uub%d" % j, "gsb%d" % blk], writes=["actT"])
                for db in range(8):
                    for kg in range(2):
                        i = wc % NWT
                        wc += 1
                        dma("sp", R(wt[i][:].rearrange("p k c -> p (k c)")), R(w_dn[ex, db, kg]), [], ["wt%d" % i], "d_wt%d" % i)

                        def md(e, i=i, kg=kg, db=db):
                            for s in range(2):
                                for kc in range(8):
                                    ins = e.matmul(pst[4 + s][:], lhsT=R(actT[:, kg * 8 + kc, s * 128:(s + 1) * 128]), rhs=R(wt[i][:, kc, :]),
                                                   start=(kg == 0 and kc == 0), stop=False)
                                if kg == 1:
                                    ins = e.matmul(pst[4 + s][:], lhsT=R(ones[0:1, :]), rhs=R(bdn[0:1, db * 512:(db + 1) * 512]),
                                                   start=False, stop=True)
                            return ins
                        P.op("pe", md, reads=["wt%d" % i, "actT", "bdn", "ones"], writes=[PK[4], PK[5]])
                    for s in range(2):
                        ob = ost[s][db % 2]
                        ok_ = "ost%d_%d" % (s, db % 2)
                        if s == 0:
                            P.op("act", lambda e, s=s, ob=ob: e.copy(out=ob[:], in_=pst[4 + s][:]), reads=[PK[4 + s]], writes=[ok_])
                        else:
                            P.op("dve", lambda e, s=s, ob=ob: e.tensor_copy(out=ob[:], in_=pst[4 + s][:]), reads=[PK[4 + s]], writes=[ok_])
                        dma("act", Ed[ex * SLOTS + s * 128: ex * SLOTS + (s + 1) * 128, db * 512:(db + 1) * 512], ob[:], [ok_], [], "d_" + ok_)
            P.barrier()
            P.emit()

        if stop_after == 'D':
            return nc
        with ExitStack() as ph:
            xa = sbt(ph, "xa", [128, D])
            gk = [sbt(ph, "gk%d" % i, [128, D]) for i in range(2)]
            gbc = sbt(ph, "gbc", [128, D])
            bbc = sbt(ph, "bbc", [128, D])
            sqt = sbt(ph, "sqt", [128, D])
            ot = sbt(ph, "ot", [128, D])
            st1 = sbt(ph, "st1", [128, 4])
            dma("sp", gbc[:], bass.AP(ln2p_h, 0, [[0, 128], [1, D]]), [], ["gbc"], "e_g")
            dma("sp", bbc[:], bass.AP(ln2p_h, D, [[0, 128], [1, D]]), [], ["bbc"], "e_b")
            gc = 0
            outs = []
            for tt in range(8):
                dma("sp", xa[:], x1d[tt * 128:(tt + 1) * 128, :], [], ["xa"], "e_x")
                P.op("dve", lambda e: e.tensor_scalar(out=xa[:], in0=xa[:], scalar1=ALPHA, scalar2=None, op0=ALU.mult),
                     reads=["xa"], writes=["xa"])
                for k in range(4):
                    i = gc % 2
                    gc += 1
                    P.op("pool", lambda e, i=i, tt=tt, k=k: e.indirect_dma_start(
                        out=gk[i][:], out_offset=None, in_=Ed,
                        in_offset=bass.IndirectOffsetOnAxis(ap=desti[:, tt, k:k + 1], axis=0)),
                        reads=["desti"], writes=["gk%d" % i], dsem="e_gk%d" % i)
                    P.op("dve", lambda e, i=i, tt=tt, k=k: e.scalar_tensor_tensor(
                        out=xa[:], in0=gk[i][:], scalar=gates[:, tt, k:k + 1], in1=xa[:], op0=ALU.mult, op1=ALU.add),
                        reads=["gk%d" % i, "xa", "gates"], writes=["xa"])
                P.op("dve", lambda e: e.reduce_sum(out=st1[:, 0:1], in_=xa[:], axis=AX.X), reads=["xa"], writes=["st0"])
                P.op("dve", lambda e: e.tensor_scalar(out=st1[:, 1:2], in0=st1[:, 0:1], scalar1=-1.0 / D, scalar2=None, op0=ALU.mult),
                     reads=["st0"], writes=["st1"])
                P.op("dve", lambda e: e.tensor_scalar(out=xa[:], in0=xa[:], scalar1=st1[:, 1:2], scalar2=None, op0=ALU.add),
                     reads=["xa", "st1"], writes=["xa"])
                P.op("act", lambda e: e.activation(out=sqt[:], in_=xa[:], func=AF.Square), reads=["xa"], writes=["sqt"])
                P.op("dve", lambda e: e.reduce_sum(out=st1[:, 2:3], in_=sqt[:], axis=AX.X), reads=["sqt"], writes=["st2"])
                P.op("dve", lambda e: e.tensor_scalar(out=st1[:, 3:4], in0=st1[:, 2:3], scalar1=1.0 / D, scalar2=EPS, op0=ALU.mult, op1=ALU.add),
                     reads=["st2"], writes=["st3"])
                rsqrt_inplace(lambda: st1[:, 3:4], "st3")
                P.op("dve", lambda e: e.scalar_tensor_tensor(out=ot[:], in0=xa[:], scalar=st1[:, 3:4], in1=gbc[:], op0=ALU.mult, op1=ALU.mult),
                     reads=["xa", "st3", "gbc"], writes=["ot"])
                P.op("pool", lambda e: e.tensor_tensor(out=ot[:], in0=ot[:], in1=bbc[:], op=ALU.add), reads=["ot", "bbc"], writes=["ot"])
                outs.append(dma("act", out[tt * 128:(tt + 1) * 128, :], ot[:], ["ot"], [], "e_out"))
            P.barrier()
            P.emit()
    return nc


def _t5_bucket_np(d):
    n = np.maximum(d, 0)
    nf = np.maximum(n, 1).astype(np.float32)
    large = 16 + (np.log(nf / np.float32(16)) / np.float32(math.log(128 / 16)) * np.float32(16)).astype(np.int32)
    large = np.minimum(large, 31)
    return np.where(n < 16, n, large)


def _tiles(xtok):
    T = xtok.shape[0]
    return np.ascontiguousarray(xtok.reshape(T // 128, 128, 32, 128).transpose(0, 3, 2, 1))


def _tile_w(w, nkg):
    E = w.shape[0]
    t = w.reshape(E, nkg, 8, 128, 8, 512).transpose(0, 4, 1, 3, 2, 5)
    return np.ascontiguousarray(t).reshape(E, 8, nkg, 128, 8 * 512)


def make_in_maps(x, mem, rel_table, w_in, w_mem_kv, w_o, lambda_q1, lambda_k1, lambda_q2, lambda_k2,
                 subln_g, conv_w, conv_b, conv_ln_g, conv_ln_b, ln1_g, ln1_b, w_router, b_router,
                 w_gate_up, b_gate_up, w_down, b_down, ln2_g, ln2_b):
    f = lambda a: np.ascontiguousarray(np.asarray(a, dtype=np.float32))
    x = f(x); mem = f(mem); rel_table = f(rel_table)
    shared = {
        "w_in": f(w_in[0]), "w_mkv": f(w_mem_kv[0]),
        "w_o": f(np.asarray(w_o[0]).reshape(32, 128, 32, 128).transpose(2, 1, 0, 3).reshape(32, 128, 4096)),
        "lamv": f(np.concatenate([lambda_q1[0], lambda_k1[0], lambda_q2[0], lambda_k2[0]])[None, :]),
        "subln": f(np.asarray(subln_g[0]).reshape(2, 128).T),
        "convw": f(np.asarray(conv_w[0])[:, 0, :].reshape(31, 8, 128).transpose(2, 1, 0)),
        "convp": f(np.stack([np.asarray(conv_b[0]).reshape(8, 128).T, np.asarray(conv_ln_g[0]).reshape(8, 128).T,
                             np.asarray(conv_ln_b[0]).reshape(8, 128).T], axis=1)),
        "ln1p": f(np.stack([np.asarray(ln1_g[0]).reshape(32, 128).T, np.asarray(ln1_b[0]).reshape(32, 128).T], axis=1)),
        "w_rt": f(w_router[0]), "b_rt": f(np.asarray(b_router[0])[None, :]),
        "w_gu": _tile_w(np.asarray(w_gate_up[0][:NEXP_UPLOAD], dtype=np.float32), 4), "b_gu": f(np.asarray(b_gate_up[0]).reshape(NE, 32, 128).transpose(2, 0, 1)),
        "w_dn": _tile_w(np.asarray(w_down[0][:NEXP_UPLOAD], dtype=np.float32), 2), "b_dn": f(b_down[0]),
        "ln2p": f(np.stack([np.asarray(ln2_g[0]), np.asarray(ln2_b[0])], axis=0)),
    }
    ext = np.concatenate([rel_table, np.full((1, 8), NEG, np.float32)], axis=0)
    in_maps = []
    for c in range(8):
        b, h = c // 2, c % 2
        xb = x[b]
        own = xb[h * 1024:(h + 1) * 1024]
        xr = xb.reshape(16, 128, D)[:, ::-1, :].reshape(2048, D)
        halo = xb[h * 1024 - 32: h * 1024] if h == 1 else np.zeros((32, D), np.float32)
        dd = np.arange(3072) - 2047 + h * 1024
        bidx = np.where(dd < 0, 32, _t5_bucket_np(dd))
        gvec = np.ascontiguousarray(ext[bidx].T)
        m = dict(shared)
        m.update({
            "xkv": _tiles(xr), "xq": _tiles(own),
            "xh": np.ascontiguousarray(halo.reshape(32, 32, 128).transpose(2, 1, 0)),
            "xqT": np.ascontiguousarray(own.T), "memt": _tiles(mem[b]), "gvec": gvec,
        })
        in_maps.append(m)
    return in_maps


def kernel(**inputs):
    in_maps = make_in_maps(**inputs)
    nc = build()
    res = run_bass_kernel_spmd(nc, in_maps, core_ids=list(range(8)))
    out = np.empty((4, 2048, D), np.float32)
    for c in range(8):
        b, h = c // 2, c % 2
        out[b, h * 1024:(h + 1) * 1024] = res.results[c]["out"]
    return out
```
